# Optimizing a Trainium2 kernel written in Bass

```python
import math
import jax, jax.numpy as jnp
from jax import lax
import numpy as np

D_MODEL = 1024
BATCH = 2
SEQ = 8192
DEPTH = 1

PLE_DIM = 256
ROPE_THETA = 10000.0
EPS = 1e-6
BLOCK = 128
NEG_INF = -1e30

SWA_HEAD_DIM = 64
SWA_HEADS = D_MODEL // SWA_HEAD_DIM
SWA_KV_HEADS = SWA_HEADS // 8
SWA_WINDOW = 128

MLA_NOPE_DIM = 128
MLA_ROPE_DIM = 64
MLA_V_DIM = 128
MLA_HEADS = D_MODEL // MLA_V_DIM
MLA_Q_RANK = 256
MLA_KV_RANK = 128

N_GROUPS = 4
EXPERTS_PER_GROUP = 4
N_EXPERTS = N_GROUPS * EXPERTS_PER_GROUP
EXPERT_TOP_K = 2
D_EXPERT = 256

IN_SIZES = (SWA_HEADS * SWA_HEAD_DIM, SWA_KV_HEADS * SWA_HEAD_DIM, SWA_KV_HEADS * SWA_HEAD_DIM,
            MLA_Q_RANK, MLA_KV_RANK, MLA_ROPE_DIM, D_MODEL, D_MODEL)
D_IN = sum(IN_SIZES)

kernel_name = "hybrid_swa_mla_hier_moe_block"


def rmsnorm(x, g):
    xf = x.astype(jnp.float32)
    r = lax.rsqrt(jnp.mean(xf * xf, axis=-1, keepdims=True) + EPS)
    return (xf * r * g.astype(jnp.float32)).astype(x.dtype)


def rope_tables(seq, dim, dtype):
    pos = jnp.arange(seq, dtype=jnp.float32)
    inv = ROPE_THETA ** (-jnp.arange(0, dim, 2, dtype=jnp.float32) / dim)
    ang = pos[:, None] * inv[None, :]
    return jnp.cos(ang).astype(dtype), jnp.sin(ang).astype(dtype)


def apply_rope(x, cos, sin):
    x1, x2 = jnp.split(x, 2, axis=-1)
    c, s = cos[:, None, :], sin[:, None, :]
    return jnp.concatenate([x1 * c - x2 * s, x2 * c + x1 * s], axis=-1)


def split_points():
    points, acc = [], 0
    for size in IN_SIZES[:-1]:
        acc += size
        points.append(acc)
    return points


def sliding_window_attention(q, k, v, sinks):
    B, S, HQ, d = q.shape
    HKV = k.shape[2]
    G = HQ // HKV
    nb = S // BLOCK
    qb = q.reshape(B, nb, BLOCK, HKV, G, d)
    kb = k.reshape(B, nb, BLOCK, HKV, d)
    vb = v.reshape(B, nb, BLOCK, HKV, d)
    pad = ((0, 0), (1, 0), (0, 0), (0, 0), (0, 0))
    kk = jnp.concatenate([jnp.pad(kb[:, :-1], pad), kb], axis=2)
    vv = jnp.concatenate([jnp.pad(vb[:, :-1], pad), vb], axis=2)
    s = jnp.einsum('bnqhgd,bnkhd->bnhgqk', qb, kk,
                   preferred_element_type=jnp.float32) * (1.0 / math.sqrt(d))
    qi = jnp.arange(BLOCK)[:, None]
    kj = jnp.arange(2 * BLOCK)[None, :]
    dist = BLOCK + qi - kj
    key_pos = jnp.arange(nb)[:, None, None] * BLOCK - BLOCK + kj[None]
    allowed = (dist >= 0)[None] & (dist < SWA_WINDOW)[None] & (key_pos >= 0)
    s = jnp.where(allowed[None, :, None, None], s, NEG_INF)
    sink = sinks.astype(jnp.float32).reshape(HKV, G)[None, None, :, :, None, None]
    m = jnp.maximum(jnp.max(s, axis=-1, keepdims=True), sink)
    e = jnp.exp(s - m)
    pr = e / (jnp.sum(e, axis=-1, keepdims=True) + jnp.exp(sink - m))
    o = jnp.einsum('bnhgqk,bnkhd->bnqhgd', pr.astype(v.dtype), vv)
    return o.reshape(B, S, HQ * d)


def latent_attention(q_nope, q_rope, k_nope, k_rope, v):
    B, S, H, _ = q_nope.shape
    nb = S // BLOCK
    qn = q_nope.reshape(B, nb, BLOCK, H, MLA_NOPE_DIM).transpose(1, 0, 2, 3, 4)
    qr = q_rope.reshape(B, nb, BLOCK, H, MLA_ROPE_DIM).transpose(1, 0, 2, 3, 4)
    starts = jnp.arange(nb, dtype=jnp.int32) * BLOCK
    kpos = jnp.arange(S, dtype=jnp.int32)
    scale = 1.0 / math.sqrt(MLA_NOPE_DIM + MLA_ROPE_DIM)

    def one_block(args):
        qn_b, qr_b, start = args
        s = (jnp.einsum('bqhd,bkhd->bhqk', qn_b, k_nope, preferred_element_type=jnp.float32)
             + jnp.einsum('bqhr,bkr->bhqk', qr_b, k_rope, preferred_element_type=jnp.float32)) * scale
        qpos = start + jnp.arange(BLOCK, dtype=jnp.int32)
        s = jnp.where(kpos[None, :] <= qpos[:, None], s, NEG_INF)
        pr = jax.nn.softmax(s, axis=-1)
        return jnp.einsum('bhqk,bkhd->bqhd', pr.astype(v.dtype), v)

    o = lax.map(one_block, (qn, qr, starts))
    return o.transpose(1, 0, 2, 3, 4).reshape(B, S, H * MLA_V_DIM)


def hybrid_mixer(xn, w_in, swa_sinks, mla_g_q, mla_w_uq, mla_g_kv, mla_w_ukv, w_out,
                 cos_a, sin_a, cos_b, sin_b):
    B, S, _ = xn.shape
    proj = xn @ w_in
    q_a, k_a, v_a, c_q, c_kv, k_rope, gate_a, gate_b = jnp.split(proj, split_points(), axis=-1)
    q_a = apply_rope(q_a.reshape(B, S, SWA_HEADS, SWA_HEAD_DIM), cos_a, sin_a)
    k_a = apply_rope(k_a.reshape(B, S, SWA_KV_HEADS, SWA_HEAD_DIM), cos_a, sin_a)
    v_a = v_a.reshape(B, S, SWA_KV_HEADS, SWA_HEAD_DIM)
    o_a = sliding_window_attention(q_a, k_a, v_a, swa_sinks)
    q = (rmsnorm(c_q, mla_g_q) @ mla_w_uq).reshape(B, S, MLA_HEADS, MLA_NOPE_DIM + MLA_ROPE_DIM)
    q_nope, q_rope = jnp.split(q, [MLA_NOPE_DIM], axis=-1)
    q_rope = apply_rope(q_rope, cos_b, sin_b)
    kv = (rmsnorm(c_kv, mla_g_kv) @ mla_w_ukv).reshape(B, S, MLA_HEADS, MLA_NOPE_DIM + MLA_V_DIM)
    k_nope, v_b = jnp.split(kv, [MLA_NOPE_DIM], axis=-1)
    k_rope = apply_rope(k_rope[:, :, None, :], cos_b, sin_b)[:, :, 0, :]
    o_b = latent_attention(q_nope, q_rope, k_nope, k_rope, v_b)
    merged = jax.nn.sigmoid(gate_a) * o_a + jax.nn.sigmoid(gate_b) * o_b
    return merged @ w_out


def hierarchical_moe(xn, w_router_group, b_router_group, w_router_expert, b_router_expert,
                     w_expert_in, w_expert_out):
    B, S, D = xn.shape
    T = B * S
    xt = xn.reshape(T, D)
    g_prob = jax.nn.softmax((xt @ w_router_group + b_router_group).astype(jnp.float32), axis=-1)
    gidx = jnp.argmax(g_prob, axis=-1)
    g_w = jnp.take_along_axis(g_prob, gidx[:, None], axis=-1)
    e_logits = (xt @ w_router_expert + b_router_expert).astype(jnp.float32)
    e_logits = e_logits.reshape(T, N_GROUPS, EXPERTS_PER_GROUP)
    e_sel = jnp.take_along_axis(e_logits, gidx[:, None, None], axis=1)[:, 0]
    e_prob = jax.nn.softmax(e_sel, axis=-1)
    top_v, top_i = lax.top_k(e_prob, EXPERT_TOP_K)
    top_v = top_v / jnp.sum(top_v, axis=-1, keepdims=True)
    w_group = jnp.sum(jax.nn.one_hot(top_i, EXPERTS_PER_GROUP, dtype=jnp.float32) * top_v[..., None], axis=1)
    combine = (jax.nn.one_hot(gidx, N_GROUPS, dtype=jnp.float32)[:, :, None]
               * w_group[:, None, :] * g_w[:, :, None]).reshape(T, N_EXPERTS)
    hid = jnp.einsum('td,edf->tef', xt, w_expert_in)
    gate, up = jnp.split(hid, 2, axis=-1)
    act = jax.nn.silu(gate) * up * combine[..., None].astype(xn.dtype)
    y = jnp.einsum('tef,efd->td', act, w_expert_out)
    return y.reshape(B, S, D)


def setup_inputs(seed: int = 0) -> dict:
    key = jax.random.key(seed)
    ks = jax.random.split(key, 24)

    def nrm(k, shape, scale):
        return jax.random.normal(k, shape, jnp.float32) * scale

    def gain(k, shape):
        return 1.0 + 0.05 * jax.random.normal(k, shape, jnp.float32)

    L = DEPTH
    return {
        "x": nrm(ks[0], (BATCH, SEQ, D_MODEL), 1.0),
        "p": nrm(ks[1], (DEPTH, BATCH, SEQ, PLE_DIM), 1.0),
        "g_mix": gain(ks[2], (L, D_MODEL)),
        "w_in": nrm(ks[3], (L, D_MODEL, D_IN), D_MODEL ** -0.5),
        "swa_sinks": nrm(ks[4], (L, SWA_HEADS), 0.5),
        "mla_g_q": gain(ks[5], (L, MLA_Q_RANK)),
        "mla_w_uq": nrm(ks[6], (L, MLA_Q_RANK, MLA_HEADS * (MLA_NOPE_DIM + MLA_ROPE_DIM)), MLA_Q_RANK ** -0.5),
        "mla_g_kv": gain(ks[7], (L, MLA_KV_RANK)),
        "mla_w_ukv": nrm(ks[8], (L, MLA_KV_RANK, MLA_HEADS * (MLA_NOPE_DIM + MLA_V_DIM)), MLA_KV_RANK ** -0.5),
        "w_out": nrm(ks[9], (L, D_MODEL, D_MODEL), D_MODEL ** -0.5),
        "g_ffn": gain(ks[10], (L, D_MODEL)),
        "w_router_group": nrm(ks[11], (L, D_MODEL, N_GROUPS), D_MODEL ** -0.5),
        "b_router_group": nrm(ks[12], (L, N_GROUPS), 0.01),
        "w_router_expert": nrm(ks[13], (L, D_MODEL, N_EXPERTS), D_MODEL ** -0.5),
        "b_router_expert": nrm(ks[14], (L, N_EXPERTS), 0.01),
        "w_expert_in": nrm(ks[15], (L, N_EXPERTS, D_MODEL, 2 * D_EXPERT), D_MODEL ** -0.5),
        "w_expert_out": nrm(ks[16], (L, N_EXPERTS, D_EXPERT, D_MODEL), D_EXPERT ** -0.5),
        "g_ple": gain(ks[17], (L, D_MODEL)),
        "w_ple_gate": nrm(ks[18], (L, D_MODEL, D_MODEL), D_MODEL ** -0.5),
        "w_ple_proj": nrm(ks[19], (L, PLE_DIM, D_MODEL), PLE_DIM ** -0.5),
        "g_final": gain(ks[20], (D_MODEL,)),
    }


def reference(x, p, g_mix, w_in, swa_sinks, mla_g_q, mla_w_uq, mla_g_kv, mla_w_ukv, w_out,
              g_ffn, w_router_group, b_router_group, w_router_expert, b_router_expert,
              w_expert_in, w_expert_out, g_ple, w_ple_gate, w_ple_proj, g_final):
    S = x.shape[1]
    cos_a, sin_a = rope_tables(S, SWA_HEAD_DIM, x.dtype)
    cos_b, sin_b = rope_tables(S, MLA_ROPE_DIM, x.dtype)
    h = x
    for i in range(DEPTH):
        h = h + hybrid_mixer(rmsnorm(h, g_mix[i]), w_in[i], swa_sinks[i], mla_g_q[i], mla_w_uq[i],
                             mla_g_kv[i], mla_w_ukv[i], w_out[i], cos_a, sin_a, cos_b, sin_b)
        h = h + hierarchical_moe(rmsnorm(h, g_ffn[i]), w_router_group[i], b_router_group[i],
                                 w_router_expert[i], b_router_expert[i], w_expert_in[i], w_expert_out[i])
        ple_gate = jax.nn.sigmoid(rmsnorm(h, g_ple[i]) @ w_ple_gate[i])
        h = h + ple_gate * (p[i] @ w_ple_proj[i])
    return rmsnorm(h, g_final)
```

```python
import numpy as np
import ml_dtypes
import concourse.bass as bass
import concourse.mybir as mybir
from concourse.bass_utils import run_bass_kernel_spmd

F32 = mybir.dt.float32
BF16 = mybir.dt.bfloat16
ALU = mybir.AluOpType
AF = mybir.ActivationFunctionType

D = 1024
SEQ = 8192
NB = 64
NOWN = 16
EPS = 1e-6
NE = 16
MLA_SCALE = 1.0 / np.sqrt(192.0)
SWA_SCALE = 1.0 / 8.0
ENG = ("pe", "act", "dve", "pool", "sp")
NSLOT = 24

DEBUG = False


class Sched:
    def __init__(self, nc, sems, dsems):
        self.nc = nc
        self.sems = sems
        self.dsems = dsems
        self.q = {e: [] for e in ENG}
        self.cnt = {e: 0 for e in ENG}
        self.seen = {e: {f: 0 for f in ENG} for e in ENG}
        self.seen_d = {e: [0] * NSLOT for e in ENG}
        self.last_w = {}
        self.readers = {}
        self.dma_q = {}
        self.slot_cnt = [0] * NSLOT

    def _waits(self, eng, reads, writes, extra=()):
        deps = list(extra)
        for k in reads:
            t = self.last_w.get(k)
            if t is not None:
                deps.append(t)
            if isinstance(k, tuple) and k[0] == "pb":
                deps.extend(r for r in self.readers.get(k, ()) if r[1] != eng)
        for k in writes:
            t = self.last_w.get(k)
            if t is not None:
                deps.append(t)
            deps.extend(self.readers.get(k, ()))
        waits = []
        for t in deps:
            if t[0] == "e":
                _, f, idx = t
                if f == eng and eng == "pe":
                    continue
                if self.seen[eng][f] >= idx:
                    continue
                self.seen[eng][f] = idx
                waits.append((self.sems[f], idx))
            else:
                _, slot, val = t
                if self.seen_d[eng][slot] >= val:
                    continue
                self.seen_d[eng][slot] = val
                waits.append((self.dsems[slot], val))
        return waits

    def _commit(self, tok, reads, writes):
        for k in writes:
            self.last_w[k] = tok
            self.readers[k] = []
        for k in reads:
            self.readers.setdefault(k, []).append(tok)

    def op(self, eng, fn, reads=(), writes=()):
        waits = self._waits(eng, reads, writes)
        self.cnt[eng] += 1
        tok = ("e", eng, self.cnt[eng])
        self.q[eng].append((waits, fn, (self.sems[eng], 1)))
        self._commit(tok, reads, writes)

    def dma(self, eng, fn, reads=(), writes=()):
        lo, n_ = (0, 16) if eng == "sp" else (16, NSLOT - 16)
        c = self.dma_q.get(eng, 0)
        self.dma_q[eng] = c + 1
        slot = lo + c % n_
        self.slot_cnt[slot] += 1
        val = 16 * self.slot_cnt[slot]
        extra = [("d", slot, val - 16)] if val > 16 else []
        waits = self._waits(eng, reads, writes, extra)
        tok = ("d", slot, val)
        self.q[eng].append((waits, fn, (self.dsems[slot], 16)))
        self._commit(tok, reads, writes)

    def barrier(self):
        for e in ENG:
            waits = []
            for f in ENG:
                if f != e and self.seen[e][f] < self.cnt[f]:
                    self.seen[e][f] = self.cnt[f]
                    waits.append((self.sems[f], self.cnt[f]))
            for s in range(NSLOT):
                n = self.slot_cnt[s]
                if n > 0 and self.seen_d[e][s] < 16 * n:
                    self.seen_d[e][s] = 16 * n
                    waits.append((self.dsems[s], 16 * n))
            if e != "pe":
                if self.seen[e][e] < self.cnt[e]:
                    self.seen[e][e] = self.cnt[e]
                    waits.append((self.sems[e], self.cnt[e]))
            self.q[e].append((waits, None, None))
        self.last_w = {}
        self.readers = {}

    def flush(self):
        nc = self.nc
        q = self.q

        def mk(name):
            def f(e):
                for waits, fn, inc in q[name]:
                    for sem, val in waits:
                        e.wait_ge(sem, val)
                    if fn is not None:
                        ins = fn(e)
                        ins.then_inc(inc[0], inc[1])
            return f

        with nc.Block() as blk:
            blk.tensor(mk("pe"))
            blk.scalar(mk("act"))
            blk.vector(mk("dve"))
            blk.gpsimd(mk("pool"))
            blk.sync(mk("sp"))
        self.q = {e: [] for e in ENG}


def build_program(stop=None):
    dbg = {}
    nc = bass.Bass("TRN2", target_bir_lowering=False)

    def din(name, shape):
        return nc.dram_tensor(name, list(shape), F32, kind="ExternalInput").ap()

    xT_full = din("xT_full", [D, SEQ])
    xT_q = din("xT_q", [D, NOWN * 128])
    xT_pair = din("xT_pair", [D, NOWN * 256])
    x_own = din("x_own", [NOWN * 128, D])
    pT_own = din("pT_own", [256, NOWN * 128])
    cs_all = din("cs_all", [SEQ, 64])
    csT_own = din("csT_own", [128, 2, NOWN * 128])
    cs_pair = din("cs_pair", [NOWN * 256, 64])
    consts = din("consts", [128, 8, 128])
    w_in = din("w_in", [D, 3776])
    w_uq = din("w_uq", [256, 1536])
    w_ukv = din("w_ukv", [128, 2048])
    w_ukvT = din("w_ukvT", [2048, 128])
    w_out = din("w_out", [D, D])
    w_r = din("w_r", [D, 20])
    b_r = din("b_r", [20])
    w_ei = din("w_ei", [NE, D, 512])
    w_eo = din("w_eo", [NE, 256, D])
    w_pg = din("w_pg", [D, D])
    w_pp = din("w_pp", [256, D])
    g_mixT = din("g_mixT", [128, 8])
    g_q = din("g_q", [256])
    g_kv = din("g_kv", [128])
    g_ffn = din("g_ffn", [D])
    g_ple = din("g_ple", [D])
    g_fin = din("g_fin", [D])
    sinks = din("sinks", [16])
    out_own = nc.dram_tensor("out_own", [NOWN * 128, D], F32, kind="ExternalOutput").ap()

    from contextlib import ExitStack

    with ExitStack() as top:
        uniq = [0]

        def sb(name, shape, dt, st=top):
            uniq[0] += 1
            return st.enter_context(nc.sbuf_tensor("%s_%d" % (name, uniq[0]), list(shape), dt))

        sems = {e: top.enter_context(nc.semaphore("s_" + e)) for e in ENG}
        dsems = [top.enter_context(nc.semaphore("d%d" % i)) for i in range(NSLOT)]
        S = Sched(nc, sems, dsems)
        pb = [top.enter_context(nc.psum_tensor("pb%d" % i, [128, 512], F32)) for i in range(8)]
        PB = lambda i: ("pb", i)

        def mm(out, lhsT, rhs, start, stop, r, w, skip=False):
            S.op("pe", lambda e: e.matmul(out, lhsT, rhs, start=start, stop=stop, skip_group_check=skip), r, w)

        def tr(out, in_, r, w):
            S.op("pe", lambda e: e.transpose(out, in_, ident), list(r) + ["consts"], w)

        def act(out, in_, func, r, w, scale=1.0, bias=None, accum=None):
            kw = {}
            if bias is not None:
                kw["bias"] = bias
            if accum is not None:
                kw["accum_out"] = accum
            S.op("act", lambda e: e.activation(out, in_, func, scale=scale, **kw), r, w)

        def tt(eng, out, a, b, op, r, w):
            S.op(eng, lambda e: e.tensor_tensor(out, a, b, op), r, w)

        def stt(eng, out, in0, scalar, in1, op0, op1, r, w):
            S.op(eng, lambda e: e.scalar_tensor_tensor(out, in0, scalar, in1, op0, op1), r, w)

        def ts(eng, out, in0, s1, s2, op0, op1, r, w):
            S.op(eng, lambda e: e.tensor_scalar(out, in0, s1, s2, op0, op1), r, w)

        def cp(eng, out, in_, r, w):
            if eng == "act":
                S.op("act", lambda e: e.copy(out, in_), r, w)
            else:
                S.op(eng, lambda e: e.tensor_copy(out, in_), r, w)

        def rcp(out, in_, r, w):
            S.op("dve", lambda e: e.reciprocal(out, in_), r, w)

        def rstd(out, in_, n, r, w):
            S.op("pool", lambda e: e.tensor_tensor(out, in_, expo[:, 0:n], ALU.pow), list(r) + ["expo"], w)

        def ms(eng, ap, val, w):
            S.op(eng, lambda e: e.memset(ap, val), (), w)

        def ld(out, in_, w, r=(), eng="sp"):
            S.dma(eng, lambda e: e.dma_start(out=out, in_=in_), r, w)

        def ldc(out, in_, w, r=()):
            S.dma("pool", lambda e: e.dma_start(out=out, in_=in_), r, w)

        def dump(name, ap, shape, dt, rkeys):
            t = nc.dram_tensor("dbg_" + name, list(shape), dt, kind="ExternalOutput").ap()
            ld(t, ap, [("dbg", name)], r=rkeys)

        def finish():
            S.barrier()
            S.flush()
            return nc

        cst = sb("cst", [128, 8, 128], BF16)
        ident = cst[:, 0, :]
        ones = sb("ones", [128, 128], BF16)
        epst = sb("epst", [128, 1], F32)
        gmix = sb("gmix", [128, 8], F32)
        esink = sb("esink", [128, 16], F32)
        o_b = sb("o_b", [128, NOWN, D], BF16)
        wB = sb("wB", [128, 8, 1280], BF16)
        wO = sb("wO", [128, 8, D], BF16)

        ldc(cst[:], consts, ["consts"])
        ms("dve", ones[:], 1.0 / 1024.0, ["ones"])
        ms("dve", epst[:], EPS, ["epst"])
        expo = sb("expo", [128, 512], F32)
        ms("pool", expo[:], -0.5, ["expo"])
        ld(gmix[:], g_mixT, ["gmix"])
        ld(esink[:], sinks.partition_broadcast(128), ["esink"])
        act(esink[:], esink[:], AF.Exp, ["esink"], ["esink"])

        def xn_keys(xn_key):
            return [(xn_key, c) for c in range(8)]

        with ExitStack() as kst:
            KT = sb("KT", [128, 2, SEQ], BF16, kst)
            Vg = sb("Vg", [128, NB, 129], BF16, kst)
            cqT = sb("cqT", [128, 2, NOWN * 128], BF16, kst)
            ms("pool", Vg[:, :, 128:129], 1.0, [("Vg", b) for b in range(NB)])
            wqn = sb("wqn", [128, 2, 8, 128], BF16, kst)
            wqr = sb("wqr", [128, 2, 8, 128], BF16, kst)
            wqt = sb("wqt", [128, 2, 8, 128], BF16, kst)
            wukT = sb("wukT", [128, 8, 128], BF16, kst)
            wuv = sb("wuv", [128, 8, 128], BF16, kst)

            with ExitStack() as ast:
                xb = sb("xb", [128, 3, 8, 512], BF16, ast)
                xsq = sb("xsq", [128, 8, 512], BF16, ast)
                csg = sb("csg", [128, 4, 4, 64], F32, ast)
                wkv = sb("wkv", [128, 8, 192], BF16, ast)
                wcq = sb("wcq", [128, 8, 256], BF16, ast)
                rsA = sb("rsA", [128, 4], F32, ast)
                r4 = sb("r4", [128, 2, 4], F32, ast)
                r2 = sb("r2", [128, 2, 4], F32, ast)
                ssy = sb("ssy", [128, 2, 4], F32, ast)
                sl = sb("sl", [128, 2, 4], F32, ast)
                junk = sb("junk", [128, 256], F32, ast)
                tA = sb("tA", [128, 2, 2, 32], F32, ast)
                tB = sb("tB", [128, 2, 2, 32], F32, ast)
                krt3 = sb("krt3", [128, 3, 128], BF16, ast)
                cqn = sb("cqn", [128, 2, 256], BF16, ast)
                gq_b = sb("gq_b", [128, 256], F32, ast)
                gkv_b = sb("gkv_b", [128, 128], F32, ast)
                ld(gq_b[:], g_q.partition_broadcast(128), ["gq_b"])
                ld(gkv_b[:], g_kv.partition_broadcast(128), ["gkv_b"])

                w_in_v = w_in.rearrange("(c p) f -> p c f", p=128)
                ldc(wkv[:], w_in_v[:, :, 1536:1728], ["wkv"])
                ldc(wcq[:], w_in_v[:, :, 1280:1536], ["wcq"])
                ms("dve", krt3[:], 0.0, [("krt", 0), ("krt", 1), ("krt", 2)])

                xTf = xT_full.rearrange("(c p) t -> p c t", p=128)
                csa = cs_all.rearrange("(b p) f -> p b f", p=128)
                xTq = xT_q.rearrange("(c p) t -> p c t", p=128)
                groups = [("kv", g) for g in range(NB // 4)] + [("cq", g) for g in range(NOWN // 4)]
                NG = len(groups)
                YB = ((1, 2), (5, 6))

                def g_load(gi):
                    kind, g = groups[gi]
                    k3 = gi % 3
                    if kind == "kv":
                        ldc(xb[:, k3], xTf[:, :, g * 512:(g + 1) * 512], [("xb", k3)])
                        ld(csg[:, gi % 4], csa[:, 4 * g:4 * g + 4, :], [("csg", gi % 4)])
                    else:
                        ldc(xb[:, k3], xTq[:, :, g * 512:(g + 1) * 512], [("xb", k3)])

                def g_sq(gi):
                    act(xsq[:], xb[:, gi % 3], AF.Square, [("xb", gi % 3)], ["xsq"])

                def g_stats(gi):
                    k = gi % 2
                    for b_ in range(4):
                        for c in range(8):
                            mm(pb[0][:, b_:b_ + 1], xsq[:, c, b_ * 128:(b_ + 1) * 128], ones[:, 0:1], b_ == 0 and c == 0, c == 7,
                               ["ones", "xsq"], [PB(0)], skip=True)
                    act(rsA[:, 0:4], pb[0][:, 0:4], AF.Identity, [PB(0), "epst"], ["rsA"], bias=epst[:, 0:1])
                    rstd(r4[:, k, :], rsA[:, 0:4], 4, ["rsA"], [("r4", k)])
                    tt("pool", r2[:, k, :], r4[:, k, :], r4[:, k, :], ALU.mult, [("r4", k)], [("r2", k)])

                def g_mm(gi):
                    kind, g = groups[gi]
                    k = gi % 2
                    k3 = gi % 3
                    w_, wkey, W, Wn = (wkv, "wkv", 192, 128) if kind == "kv" else (wcq, "wcq", 256, 256)
                    ms("dve", ssy[:, k, :], 0.0, [("ssy", k)])
                    for b_ in range(4):
                        bank = YB[k][b_ // 2]
                        off = (b_ % 2) * W
                        for c in range(8):
                            mm(pb[bank][:, off:off + W], xb[:, k3, c, b_ * 128:(b_ + 1) * 128], w_[:, c, :],
                               b_ % 2 == 0 and c == 0, c == 7, [("xb", k3), wkey], [PB(bank)], skip=True)
                    for b_ in range(4):
                        bank = YB[k][b_ // 2]
                        off = (b_ % 2) * W
                        act(junk[:, 0:Wn], pb[bank][:, off:off + Wn], AF.Square, [PB(bank)], ["junk", ("ssy", k)],
                            accum=ssy[:, k, b_:b_ + 1], scale=float(Wn ** -0.5))

                def g_sc_a(gi):
                    k = gi % 2
                    tt("dve", sl[:, k, :], ssy[:, k, :], r2[:, k, :], ALU.mult, [("ssy", k), ("r2", k)], [("sl", k)])
                    ts("dve", sl[:, k, :], sl[:, k, :], EPS, None, ALU.add, ALU.bypass, [("sl", k)], [("sl", k)])
                    rstd(sl[:, k, :], sl[:, k, :], 4, [("sl", k)], [("sl", k)])

                def g_sc_b(gi):
                    k = gi % 2
                    tt("dve", sl[:, k, :], sl[:, k, :], r4[:, k, :], ALU.mult, [("sl", k), ("r4", k)], [("sl", k)])

                def g_ev(gi, b_):
                    kind, g = groups[gi]
                    k = gi % 2
                    t = 4 * gi + b_
                    kk = t % 2
                    k3 = t % 3
                    blk = 4 * g + b_
                    bank = YB[k][b_ // 2]
                    if kind == "kv":
                        off = (b_ % 2) * 192
                        stt("dve", Vg[:, blk, 0:128], pb[bank][:, off:off + 128], sl[:, k, b_:b_ + 1], gkv_b[:],
                            ALU.mult, ALU.mult, [PB(bank), ("sl", k), "gkv_b"], [("Vg", blk)])
                        kr = pb[bank][:, off + 128:off + 192].rearrange("p (t f) -> p t f", t=2)
                        cosb = csg[:, gi % 4, b_, 0:32].unsqueeze(1).to_broadcast([128, 2, 32])
                        sinb = csg[:, gi % 4, b_, 32:64].unsqueeze(1).to_broadcast([128, 2, 32])
                        stt("dve", tA[:, kk], kr, r4[:, k, b_:b_ + 1], cosb, ALU.mult, ALU.mult,
                            [PB(bank), ("csg", gi % 4), ("r4", k)], [("tA", kk)])
                        stt("dve", tB[:, kk], kr, r4[:, k, b_:b_ + 1], sinb, ALU.mult, ALU.mult,
                            [PB(bank), ("csg", gi % 4), ("r4", k)], [("tB", kk)])
                        tt("dve", krt3[:, k3, 0:32], tA[:, kk, 0, :], tB[:, kk, 1, :], ALU.subtract,
                           [("tA", kk), ("tB", kk)], [("krt", k3)])
                        tt("dve", krt3[:, k3, 32:64], tA[:, kk, 1, :], tB[:, kk, 0, :], ALU.add,
                           [("tA", kk), ("tB", kk)], [("krt", k3)])
                    else:
                        off = (b_ % 2) * 256
                        stt("dve", cqn[:, kk, :], pb[bank][:, off:off + 256], sl[:, k, b_:b_ + 1], gq_b[:], ALU.mult, ALU.mult,
                            [PB(bank), ("sl", k), "gq_b"], [("cqn", kk)])

                def g_tr(gi, b_):
                    kind, g = groups[gi]
                    t = 4 * gi + b_
                    kk = t % 2
                    k3 = t % 3
                    blk = 4 * g + b_
                    tbank = 3 + kk
                    tp = pb[tbank][:].bitcast(BF16)
                    if kind == "kv":
                        tr(tp[:, 0:128], Vg[:, blk, 0:128], [("Vg", blk)], [PB(tbank)])
                        tr(tp[:, 128:256], krt3[:, k3, :], [("krt", k3)], [PB(tbank)])
                        cp("dve" if b_ % 2 == 0 else "act", KT[:, :, blk * 128:(blk + 1) * 128],
                           tp[:, 0:256].rearrange("p (t f) -> p t f", t=2), [PB(tbank)], [("KT", blk)])
                    else:
                        tr(tp[:, 0:128], cqn[:, kk, 0:128], [("cqn", kk)], [PB(tbank)])
                        tr(tp[:, 128:256], cqn[:, kk, 128:256], [("cqn", kk)], [PB(tbank)])
                        cp("dve" if b_ % 2 == 0 else "act", cqT[:, :, blk * 128:(blk + 1) * 128],
                           tp[:, 0:256].rearrange("p (t f) -> p t f", t=2), [PB(tbank)], [("cqT", blk)])

                g_load(0)
                g_load(1)
                g_load(2)
                wq_v = w_uq.rearrange("(c p) (h f) -> p c h f", p=128, f=192)
                for c in range(2):
                    ldc(wqn[:, c], wq_v[:, c, :, 0:128], ["wqn"])
                ms("pool", wqr[:], 0.0, ["wqr"])
                ms("pool", wqt[:], 0.0, ["wqt"])
                for c in range(2):
                    ldc(wqr[:, c, :, 0:64], wq_v[:, c, :, 128:192], ["wqr"])
                ldc(wukT[:], w_ukvT.rearrange("(h t n) r -> n h t r", t=2, n=128)[:, :, 0, :], ["wukT"])
                ldc(wuv[:], w_ukv.rearrange("r (h t v) -> r h t v", t=2, v=128)[:, :, 1, :], ["wuv"])
                for c in range(8):
                    ts("dve", wkv[:, c, :], wkv[:, c, :], gmix[:, c:c + 1], None, ALU.mult, ALU.bypass, ["wkv", "gmix"], ["wkv"])
                    ts("dve", wcq[:, c, :], wcq[:, c, :], gmix[:, c:c + 1], None, ALU.mult, ALU.bypass, ["wcq", "gmix"], ["wcq"])
                g_sq(0)
                g_stats(0)
                g_mm(0)
                g_sc_a(0)
                g_sq(1)
                g_sc_b(0)
                g_stats(1)
                for gi in range(NG):
                    if gi == 4:
                        ts("pool", wqt[:, :, :, 0:32], wqr[:, :, :, 32:64], -1.0, None, ALU.mult, ALU.bypass, ["wqr"], ["wqt"])
                        cp("pool", wqt[:, :, :, 32:64], wqr[:, :, :, 0:32], ["wqr"], ["wqt"])
                    if gi + 3 < NG:
                        g_load(gi + 3)
                    if gi + 2 < NG:
                        g_sq(gi + 2)
                    if gi + 1 < NG:
                        g_mm(gi + 1)
                    for b_ in range(5):
                        if b_ < 4:
                            g_ev(gi, b_)
                        if b_ >= 1:
                            g_tr(gi, b_ - 1)
                    if gi + 1 < NG:
                        g_sc_a(gi + 1)
                    if gi + 2 < NG:
                        g_stats(gi + 2)
                    if gi + 1 < NG:
                        g_sc_b(gi + 1)
                if stop == "A":
                    dump("KT", KT[:], [128, 2, SEQ], BF16, [("KT", b) for b in range(NB)])
                    dump("Vg", Vg[:], [128, NB, 129], BF16, [("Vg", b) for b in range(NB)])
                    dump("cqT", cqT[:], [128, 2, NOWN * 128], BF16, [("cqT", b) for b in range(NOWN)])
                    return finish()
                S.barrier()
                S.flush()

            with ExitStack() as mst:
                qabs = sb("qabs", [128, 2, 8, 512], BF16, mst)
                qrope = sb("qrope", [128, 2, 8, 512], BF16, mst)
                qn_sb = sb("qn_sb", [128, 2, 512], BF16, mst)
                csT = sb("csT", [128, 2, 2, 512], F32, mst)
                t1 = sb("t1", [128, 2, 512], F32, mst)
                t2 = sb("t2", [128, 2, 512], F32, mst)
                pT = sb("pT", [128, 4, 512], BF16, mst)
                rc = sb("rc", [128, 8], F32, mst)
                olat = sb("olat", [128, 2, D], BF16, mst)
                olT = sb("olT", [128, 2, D], BF16, mst)

                ldc(wB[:], w_in_v[:, :, 0:1280], ["wB"])
                ldc(wO[:], w_out.rearrange("(c p) f -> p c f", p=128), ["wO"])

                LOOK = 2

                def qprep(ig, h, part):
                    qk = ig % 2
                    cols = slice(ig * 512, (ig + 1) * 512)
                    hk2 = h % 2
                    cq_keys = [("cqT", 4 * ig + b_) for b_ in range(4)]
                    if part == 0:
                        if h == 0:
                            ld(csT[:, qk], csT_own[:, :, cols], [("csT", qk)])
                        for c in range(2):
                            mm(pb[6][:], wqn[:, c, h, :], cqT[:, c, cols], c == 0, c == 1, ["wqn"] + cq_keys, [PB(6)])
                        cp("dve", qn_sb[:, hk2, :], pb[6][:], [PB(6)], [("qn_sb", hk2)])
                    elif part == 1:
                        for c in range(2):
                            mm(pb[7][:], wqr[:, c, h, :], cqT[:, c, cols], c == 0, c == 1, ["wqr"] + cq_keys, [PB(7)])
                        tt("dve", t1[:, hk2, :], pb[7][:], csT[:, qk, 0, :], ALU.mult, [PB(7), ("csT", qk)], [("t1", hk2)])
                    elif part == 2:
                        mm(pb[6][:], wukT[:, h, :], qn_sb[:, hk2, :], True, True, ["wukT", ("qn_sb", hk2)], [PB(6)])
                        cp("dve", qabs[:, qk, h, :], pb[6][:], [PB(6)], [("qabs", qk, h)])
                    else:
                        for c in range(2):
                            mm(pb[7][:], wqt[:, c, h, :], cqT[:, c, cols], c == 0, c == 1, ["wqt"] + cq_keys, [PB(7)])
                        tt("dve", t2[:, hk2, :], pb[7][:], csT[:, qk, 1, :], ALU.mult, [PB(7), ("csT", qk)], [("t2", hk2)])
                        tt("pool", qrope[:, qk, h, :], t1[:, hk2, :], t2[:, hk2, :], ALU.add,
                           [("t1", hk2), ("t2", hk2)], [("qrope", qk, h)])

                def evac_a(i):
                    ok = i % 2
                    for bk in range(3):
                        nh = 3 if bk < 2 else 2
                        ov = pb[3 + bk][:, 0:nh * 129].rearrange("p (h f) -> p h f", f=129)
                        rcp(rc[:, 3 * bk:3 * bk + nh], ov[:, :, 128], [PB(3 + bk)], ["rc"])
                        tt("dve", olat[:, ok, 384 * bk:384 * bk + nh * 128].rearrange("p (h f) -> p h f", f=128),
                           ov[:, :, 0:128], rc[:, 3 * bk:3 * bk + nh].unsqueeze(2).to_broadcast([128, nh, 128]),
                           ALU.mult, [PB(3 + bk), "rc"], [("olat", ok)])

                def evac_b(i):
                    ok = i % 2
                    tp = pb[6][:].bitcast(BF16)
                    for h in range(8):
                        tr(tp[:, h * 128:(h + 1) * 128], olat[:, ok, h * 128:(h + 1) * 128], [("olat", ok)], [PB(6)])
                    cp("dve", olT[:, ok, :], tp[:, :], [PB(6)], [("olT", ok)])

                def evac_c(i, part):
                    ok = i % 2
                    for h in range(4 * part, 4 * part + 4):
                        mm(pb[7][:, (h % 4) * 128:(h % 4 + 1) * 128], olT[:, ok, h * 128:(h + 1) * 128], wuv[:, h, :],
                           True, True, [("olT", ok), "wuv"], [PB(7)])
                    cp("dve", o_b[:, i, 512 * part:512 * (part + 1)], pb[7][:], [PB(7)], [("o_b", i)])

                steps = []
                for i in range(NOWN):
                    for kb in range(4 * i + 4):
                        for hg in range(2):
                            steps.append((i, kb, hg))
                NS_ = len(steps)
                deferred = {}

                def defer(at, fn):
                    deferred.setdefault(min(at, NS_ - 1), []).append(fn)

                for h in range(8):
                    for part in range(4):
                        qprep(0, h, part)
                first_of_ig = {}
                for n, (i, kb, hg) in enumerate(steps):
                    if kb == 0 and hg == 0 and i % 4 == 0:
                        first_of_ig[i // 4] = n
                for ig in range(1, 4):
                    n0 = first_of_ig[ig - 1] + 4
                    for h in range(8):
                        for part in range(4):
                            defer(n0 + 2 * (4 * h + part), (lambda ig=ig, h=h, part=part: qprep(ig, h, part)))

                def s_stage(n):
                    i, kb, hg = steps[n]
                    qk = (i // 4) % 2
                    qc = slice((i % 4) * 128, (i % 4 + 1) * 128)
                    kcol = slice(kb * 128, (kb + 1) * 128)
                    sbk = n % 3
                    pk = n % 4
                    qkeys = [("qabs", qk, h) for h in range(4 * hg, 4 * hg + 4)]
                    rkeys = [("qrope", qk, h) for h in range(4 * hg, 4 * hg + 4)]
                    mm(pb[sbk][:].rearrange("p (h q) -> p h q", h=4), KT[:, 0, kcol], qabs[:, qk, 4 * hg:4 * hg + 4, qc], True, False,
                       [("KT", kb)] + qkeys, [PB(sbk)])
                    mm(pb[sbk][:].rearrange("p (h q) -> p h q", h=4), KT[:, 1, kcol], qrope[:, qk, 4 * hg:4 * hg + 4, qc], False, True,
                       [("KT", kb)] + rkeys, [PB(sbk)])
                    act(pT[:, pk, :], pb[sbk][:], AF.Exp, [PB(sbk)], [("pT", pk)], scale=float(MLA_SCALE))
                    if kb >= 4 * i:
                        m = kb - 4 * i
                        pv = pT[:, pk, :].rearrange("p (h q) -> p h q", h=4)
                        tt("dve", pv, pv, cst[:, 1 + m, :].unsqueeze(1).to_broadcast([128, 4, 128]), ALU.mult,
                           [("pT", pk), "consts"], [("pT", pk)])

                def p_stage(n):
                    i, kb, hg = steps[n]
                    nkb = 4 * i + 4
                    pk = n % 4
                    for hh in range(4):
                        h = 4 * hg + hh
                        ob = 3 + h // 3
                        oc = (h % 3) * 129
                        mm(pb[ob][:, oc:oc + 129], pT[:, pk, hh * 128:(hh + 1) * 128], Vg[:, kb, :],
                           kb == 0 and h % 3 == 0, kb == nkb - 1, [("pT", pk), ("Vg", kb)], [PB(ob)], skip=True)
                    if kb == nkb - 1 and hg == 1:
                        evac_a(i)
                        defer(n + 3, lambda i=i: evac_b(i))
                        defer(n + 5, lambda i=i: evac_c(i, 0))
                        defer(n + 7, lambda i=i: evac_c(i, 1))

                for n in range(NS_ + LOOK):
                    if n < NS_:
                        s_stage(n)
                    m_ = n - LOOK
                    if m_ >= 0:
                        p_stage(m_)
                        for fn in deferred.pop(m_, []):
                            fn()
                for k_ in sorted(deferred):
                    for fn in deferred[k_]:
                        fn()
                for c in range(8):
                    ts("dve", wB[:, c, :], wB[:, c, :], gmix[:, c:c + 1], None, ALU.mult, ALU.bypass, ["wB", "gmix"], ["wB"])
                if stop == "M":
                    dump("o_b", o_b[:], [128, NOWN, D], BF16, [("o_b", b) for b in range(NOWN)])
                    return finish()
                S.barrier()
                S.flush()

        wG = sb("wG", [128, 8, 2048], BF16)
        for half in range(2):
            with ExitStack() as hst:
                hres = sb("hres", [128, 8, D], F32, hst)
                with ExitStack() as gst:
                    xpb = sb("xpb", [128, 2, 8, 256], BF16, gst)
                    rr = sb("rr", [128, 2, 4], F32, gst)
                    csp = sb("csp", [128, 2, 2, 64], F32, gst)
                    xsq = sb("xsq2", [128, 8, 256], BF16, gst)
                    rs = sb("rs2", [128, 4], F32, gst)
                    qA = sb("qA", [128, D], F32, gst)
                    qB = sb("qB", [128, D], F32, gst)
                    qar = sb("qar", [128, D], BF16, gst)
                    kA = sb("kA", [128, 2, 128], F32, gst)
                    kB = sb("kB", [128, 2, 128], F32, gst)
                    kpad = sb("kpad", [128, 2, 4, 128], BF16, gst)
                    vaug = sb("vaug", [128, 2, 2, 65], BF16, gst)
                    qaT = sb("qaT", [128, 8, 128], BF16, gst)
                    kT = sb("kT", [128, 8, 128], BF16, gst)
                    pS = sb("pS", [128, 8, 512], BF16, gst)
                    den = sb("den", [128, 16], F32, gst)
                    oa = sb("oa", [128, 16, 64], F32, gst)
                    m1 = sb("m1", [128, D], BF16, gst)
                    m2 = sb("m2", [128, D], BF16, gst)
                    mg = sb("mg", [128, D], BF16, gst)
                    mgT = sb("mgT", [128, 8, 128], BF16, gst)

                    w_in_v = w_in.rearrange("(c p) f -> p c f", p=128)
                    if half == 0:
                        for c in range(8):
                            ldc(wG[:, c, :], w_in_v[:, c, 1728:3776], [("wG", c)])
                        for c in range(8):
                            ts("dve", wG[:, c, :], wG[:, c, :], gmix[:, c:c + 1], None, ALU.mult, ALU.bypass,
                               [("wG", c), "gmix"], [("wG", c)])
                    ms("pool", kpad[:], 0.0, [("kpad", t_, p_) for t_ in range(2) for p_ in range(2)])
                    ms("pool", vaug[:, :, :, 64:65], 1.0, ["vaug"])
                    xTp = xT_pair.rearrange("(c p) t -> p c t", p=128)
                    csp_v = cs_pair.rearrange("(b p) f -> p b f", p=128)
                    th2 = sb("th2", [128, 2, 2048], BF16, gst)

                    def pg_load(ii):
                        i = half * 8 + ii
                        k = ii % 2
                        ldc(xpb[:, k], xTp[:, :, i * 256:(i + 1) * 256], [("xpb", k)])
                        ld(csp[:, k], csp_v[:, 2 * i:2 * i + 2, :], [("csp", k)])
                        ld(hres[:, ii, :], x_own[i * 128:(i + 1) * 128, :], [("hres", ii)])

                    def pg_sq(ii):
                        k = ii % 2
                        act(xsq[:], xpb[:, k], AF.Square, [("xpb", k)], ["xsq"])

                    def pg_x1a(ii):
                        k = ii % 2
                        for t in range(2):
                            for c in range(8):
                                mm(pb[1][:, t:t + 1], xsq[:, c, t * 128:(t + 1) * 128], ones[:, 0:1], t == 0 and c == 0, c == 7,
                                   ["ones", "xsq"], [PB(1)], skip=True)
                        act(rs[:, 0:2], pb[1][:, 0:2], AF.Identity, [PB(1), "epst"], ["rs"], bias=epst[:, 0:1])
                        rstd(rr[:, k, 0:2], rs[:, 0:2], 2, ["rs"], [("rr", k)])
                        ts("pool", rr[:, k, 2:3], rr[:, k, 1:2], 0.5, None, ALU.mult, ALU.bypass, [("rr", k)], [("rr", k)])
                        if ii + 1 < 8:
                            pg_load(ii + 1)

                    def pg_gate(ii, q4, parts=(0, 1), bank=None):
                        k = ii % 2
                        gb = 6 + (q4 % 2) if bank is None else bank
                        for part in parts:
                            for c in range(4 * part, 4 * part + 4):
                                mm(pb[gb][:], xpb[:, k, c, 128:256], wG[:, c, q4 * 512:(q4 + 1) * 512], c == 0, c == 7,
                                   [("xpb", k), ("wG", c)], [PB(gb)])
                            if part == 1:
                                act(th2[:, k, q4 * 512:(q4 + 1) * 512], pb[gb][:], AF.Tanh, [PB(gb), ("rr", k)], [("th", k, q4)],
                                    scale=rr[:, k, 2:3])

                    def pg_x2(ii):
                        k = ii % 2
                        for t in range(2):
                            for c in range(8):
                                mm(pb[5][:, t * 256:(t + 1) * 256], xpb[:, k, c, t * 128:(t + 1) * 128], wB[:, c, 1024:1280],
                                   c == 0, c == 7, [("xpb", k), "wB"], [PB(5)])
                        for hf in range(2):
                            for c in range(8):
                                mm(pb[6 + hf][:], xpb[:, k, c, 128:256], wB[:, c, hf * 512:(hf + 1) * 512], c == 0, c == 7,
                                   [("xpb", k), "wB"], [PB(6 + hf)])
                        m3 = lambda a: a.rearrange("p h t f -> p (h t) f")
                        for t in range(2):
                            kv = pb[5][:, t * 256:t * 256 + 128].rearrange("p (h t f) -> p h t f", h=2, t=2)
                            kAv = kA[:, t, :].rearrange("p (h t f) -> p h t f", h=2, t=2)
                            kBv = kB[:, t, :].rearrange("p (h t f) -> p h t f", h=2, t=2)
                            cos3 = csp[:, k, t, 0:32].unsqueeze(1).to_broadcast([128, 4, 32])
                            sin3 = csp[:, k, t, 32:64].unsqueeze(1).to_broadcast([128, 4, 32])
                            stt("dve", m3(kAv), m3(kv), rr[:, k, t:t + 1], cos3, ALU.mult, ALU.mult,
                                [PB(5), ("csp", k), ("rr", k)], [("kA", t)])
                            stt("dve", m3(kBv), m3(kv), rr[:, k, t:t + 1], sin3, ALU.mult, ALU.mult,
                                [PB(5), ("csp", k), ("rr", k)], [("kB", t)])
                            ts("dve", vaug[:, t, :, 0:64], pb[5][:, t * 256 + 128:(t + 1) * 256].rearrange("p (h f) -> p h f", h=2),
                               rr[:, k, t:t + 1], None, ALU.mult, ALU.bypass, [PB(5), ("rr", k)], ["vaug"])
                            kp5 = kpad[:, t].rearrange("p (hk par) d -> p hk par d", par=2)
                            for par in range(2):
                                kpv = kp5[:, :, par, par * 64:(par + 1) * 64].rearrange("p h (t f) -> p h t f", t=2)
                                tt("dve", kpv[:, :, 0, :], kAv[:, :, 0, :], kBv[:, :, 1, :], ALU.subtract,
                                   [("kA", t), ("kB", t)], [("kpad", t, par)])
                                tt("dve", kpv[:, :, 1, :], kAv[:, :, 1, :], kBv[:, :, 0, :], ALU.add,
                                   [("kA", t), ("kB", t)], [("kpad", t, par)])
                        cos3 = csp[:, k, 1, 0:32].unsqueeze(1).to_broadcast([128, 16, 32])
                        sin3 = csp[:, k, 1, 32:64].unsqueeze(1).to_broadcast([128, 16, 32])
                        for hf in range(2):
                            qv = pb[6 + hf][:].rearrange("p (h t f) -> p h t f", h=8, t=2)
                            qAv = qA[:, hf * 512:(hf + 1) * 512].rearrange("p (h t f) -> p h t f", h=8, t=2)
                            qBv = qB[:, hf * 512:(hf + 1) * 512].rearrange("p (h t f) -> p h t f", h=8, t=2)
                            stt("dve", m3(qAv), m3(qv), rr[:, k, 1:2], cos3, ALU.mult, ALU.mult,
                                [PB(6 + hf), ("csp", k), ("rr", k)], [("qA", hf)])
                            stt("dve", m3(qBv), m3(qv), rr[:, k, 1:2], sin3, ALU.mult, ALU.mult,
                                [PB(6 + hf), ("csp", k), ("rr", k)], [("qB", hf)])
                        qA4 = qA[:].rearrange("p (h t f) -> p h t f", h=16, t=2)
                        qB4 = qB[:].rearrange("p (h t f) -> p h t f", h=16, t=2)
                        qar4 = qar[:].rearrange("p (h t f) -> p h t f", h=16, t=2)
                        tt("dve", qar4[:, :, 0, :], qA4[:, :, 0, :], qB4[:, :, 1, :], ALU.subtract,
                           [("qA", 0), ("qA", 1), ("qB", 0), ("qB", 1)], ["qar"])
                        tt("dve", qar4[:, :, 1, :], qA4[:, :, 1, :], qB4[:, :, 0, :], ALU.add,
                           [("qA", 0), ("qA", 1), ("qB", 0), ("qB", 1)], ["qar"])

                    def pg_x3(ii, gates=False):
                        tp2 = pb[5][:].bitcast(BF16)
                        for t in range(2):
                            for v in range(4):
                                tr(tp2[:, (t * 4 + v) * 128:(t * 4 + v + 1) * 128], kpad[:, t, v, :], [("kpad", t, v % 2)], [PB(5)])
                        cp("dve", kT[:].rearrange("p c f -> p (c f)"), tp2[:, :], [PB(5)], ["kT"])
                        if gates:
                            pg_gate(ii, 2)
                        tp = pb[0][:].bitcast(BF16)
                        for c in range(8):
                            tr(tp[:, c * 128:(c + 1) * 128], qar[:, c * 128:(c + 1) * 128], ["qar"], [PB(0)])
                        cp("act", qaT[:].rearrange("p c f -> p (c f)"), tp[:, :], [PB(0)], ["qaT"])
                        if gates:
                            pg_gate(ii, 3)

                    def pg_y1(ii, nxt):
                        i = half * 8 + ii
                        sidx = 0
                        for hk in range(2):
                            for par in range(2):
                                for t in range(2):
                                    sbk = sidx % 2
                                    mm(pb[sbk][:].rearrange("p (h q) -> p h q", h=4), kT[:, t * 4 + hk * 2 + par, :],
                                       qaT[:, 4 * hk:4 * hk + 4, :], True, True, ["kT", "qaT"], [PB(sbk)])
                                    act(pS[:, sidx, :], pb[sbk][:], AF.Exp, [PB(sbk)], [("pS", sidx)], scale=float(SWA_SCALE))
                                    mi = 7 if t == 1 else (5 if i == 0 else 6)
                                    pv = pS[:, sidx, :].rearrange("p (h q) -> p h q", h=4)
                                    tt("dve", pv, pv,
                                       cst[:, mi, :].unsqueeze(1).to_broadcast([128, 4, 128]), ALU.mult,
                                       [("pS", sidx), "consts"], [("pS", sidx)])
                                    sidx += 1
                                    if nxt and sidx in (2, 4):
                                        pg_gate(ii + 1, 0, parts=(sidx // 2 - 1,))

                    def pg_y2(ii):
                        i = half * 8 + ii
                        k = ii % 2
                        started = set()
                        sidx = 0
                        for hk in range(2):
                            for par in range(2):
                                for t in range(2):
                                    for ci in range(4):
                                        head = 2 * (4 * hk + ci) + par
                                        ob = 2 + head // 7
                                        oc = (head % 7) * 65
                                        first = ob not in started
                                        started.add(ob)
                                        mm(pb[ob][:, oc:oc + 65], pS[:, sidx, ci * 128:(ci + 1) * 128], vaug[:, t, hk, :],
                                           first, t == 1, [("pS", sidx), "vaug"], [PB(ob)], skip=True)
                                    sidx += 1
                        for bk in range(3):
                            nh = 7 if bk < 2 else 2
                            ov = pb[2 + bk][:, 0:nh * 65].rearrange("p (h f) -> p h f", f=65)
                            tt("dve", den[:, 7 * bk:7 * bk + nh], ov[:, :, 64], esink[:, 7 * bk:7 * bk + nh], ALU.add,
                               [PB(2 + bk), "esink"], [("den", bk)])
                            rcp(den[:, 7 * bk:7 * bk + nh], den[:, 7 * bk:7 * bk + nh], [("den", bk)], [("den", bk)])
                            tt("dve", oa[:, 7 * bk:7 * bk + nh, :], ov[:, :, 0:64],
                               den[:, 7 * bk:7 * bk + nh].unsqueeze(2).to_broadcast([128, nh, 64]), ALU.mult,
                               [PB(2 + bk), ("den", bk)], [("oa", bk)])
                        oaf = oa[:].rearrange("p h f -> p (h f)")
                        stt("dve", m1[:], th2[:, k, 0:1024], 1.0, oaf, ALU.add, ALU.mult,
                            [("th", k, 0), ("th", k, 1), ("oa", 0), ("oa", 1), ("oa", 2)], ["m1"])
                        stt("dve", m2[:], th2[:, k, 1024:2048], 1.0, o_b[:, i, :], ALU.add, ALU.mult,
                            [("th", k, 2), ("th", k, 3), ("o_b", i)], ["m2"])
                        tt("dve", mg[:], m1[:], m2[:], ALU.add, ["m1", "m2"], ["mg"])

                    def pg_y3(ii, nxt=False):
                        if nxt:
                            pg_gate(ii + 1, 1, parts=(0,), bank=0)
                        tp = pb[1][:].bitcast(BF16)
                        for c in range(8):
                            tr(tp[:, c * 128:(c + 1) * 128], mg[:, c * 128:(c + 1) * 128], ["mg"], [PB(1)])
                        cp("act", mgT[:].rearrange("p c f -> p (c f)"), tp[:, :], [PB(1)], ["mgT"])
                        if nxt:
                            pg_gate(ii + 1, 1, parts=(1,), bank=0)

                    def pg_y4(ii):
                        for hf in range(2):
                            for c in range(8):
                                mm(pb[2 + hf][:], mgT[:, c, :], wO[:, c, hf * 512:(hf + 1) * 512], c == 0, c == 7,
                                   ["mgT", "wO"], [PB(2 + hf)])
                            stt("dve", hres[:, ii, hf * 512:(hf + 1) * 512], pb[2 + hf][:], 0.5,
                                hres[:, ii, hf * 512:(hf + 1) * 512], ALU.mult, ALU.add,
                                [PB(2 + hf), ("hres", ii)], [("hres", ii)])

                    pg_load(0)
                    pg_sq(0)
                    pg_x1a(0)
                    for q4 in range(4):
                        pg_gate(0, q4)
                    pg_x2(0)
                    pg_x3(0)
                    pg_sq(1)
                    for ii in range(8):
                        nxt = ii + 1 < 8
                        if nxt:
                            pg_x1a(ii + 1)
                        pg_y1(ii, nxt)
                        pg_y2(ii)
                        if ii + 2 < 8:
                            pg_sq(ii + 2)
                        if nxt:
                            pg_x2(ii + 1)
                        pg_y3(ii, nxt)
                        pg_y4(ii)
                        if nxt:
                            pg_x3(ii + 1, gates=True)
                    if stop == "G" and half == 0:
                        dump("hres", hres[:], [128, 8, D], F32, [("hres", b) for b in range(8)])
                        return finish()
                    S.barrier()
                    S.flush()

                with ExitStack() as est:
                    hnT = sb("hnT", [128, 8, 8 * 128], BF16, est)
                    hn = sb("hn", [128, 2, D], BF16, est)
                    junk2 = sb("junk2", [128, D], F32, est)
                    ssh = sb("ssh", [128, 8], F32, est)
                    wr = sb("wr", [128, 8, 20], BF16, est)
                    lg = sb("lg", [128, 8, 20], F32, est)
                    gmx = sb("gmx", [128, 8], F32, est)
                    oh = sb("oh", [128, 8, 4], F32, est)
                    ge = sb("ge", [128, 8, 4], F32, est)
                    gs = sb("gs", [128, 8], F32, est)
                    esel4 = sb("esel4", [128, 8, 4, 4], F32, est)
                    esel = sb("esel", [128, 8, 4], F32, est)
                    e1 = sb("e1", [128, 8], F32, est)
                    em = sb("em", [128, 8, 4], F32, est)
                    e2 = sb("e2", [128, 8], F32, est)
                    sel = sb("sel", [128, 8, 4], F32, est)
                    ew = sb("ew", [128, 8, 4], F32, est)
                    es = sb("es", [128, 8], F32, est)
                    cmb = sb("cmb", [128, 8, 16], F32, est)
                    wei = sb("wei", [128, 2, 8, 512], BF16, est)
                    weo = sb("weo", [128, 2, 2, D], BF16, est)
                    sg = sb("sg", [128, 2, 256], F32, est)
                    ac = sb("ac", [128, 2, 256], BF16, est)
                    acT = sb("acT", [128, 2, 256], BF16, est)
                    gffn_b = sb("gffn_b", [128, D], F32, est)
                    br_b = sb("br_b", [128, 20], F32, est)
                    ld(gffn_b[:], g_ffn.partition_broadcast(128), ["gffn_b"])
                    ld(br_b[:], b_r.partition_broadcast(128), ["br_b"])

                    ldc(wr[:], w_r.rearrange("(c p) f -> p c f", p=128), ["wr"])
                    wei_v = w_ei.rearrange("e (c p) f -> e p c f", p=128)
                    weo_v = w_eo.rearrange("e (c p) f -> e p c f", p=128)
                    for ex_ in range(2):
                        ldc(wei[:, ex_], wei_v[ex_], [("wei", ex_)])
                        ldc(weo[:, ex_], weo_v[ex_], [("weo", ex_)])
                    ms("dve", ssh[:], EPS, [("ssh", ii_) for ii_ in range(8)])

                    def pre_n(ii):
                        kk = ii % 2
                        act(junk2[:], hres[:, ii, :], AF.Square, [("hres", ii)], ["junk2", ("ssh", ii)], accum=ssh[:, ii:ii + 1],
                            scale=float(D ** -0.5))
                        rstd(ssh[:, ii:ii + 1], ssh[:, ii:ii + 1], 1, [("ssh", ii)], [("ssh", ii)])
                        stt("dve", hn[:, kk, :], hres[:, ii, :], ssh[:, ii:ii + 1], gffn_b[:], ALU.mult, ALU.mult,
                            [("hres", ii), ("ssh", ii), "gffn_b"], [("hn", kk)])

                    def pre_t(ii):
                        kk = ii % 2
                        tbank = 4 + kk
                        tp = pb[tbank][:].bitcast(BF16)
                        for c in range(8):
                            tr(tp[:, c * 128:(c + 1) * 128], hn[:, kk, c * 128:(c + 1) * 128], [("hn", kk)], [PB(tbank)])
                        cp("act", hnT[:, :, ii * 128:(ii + 1) * 128], tp[:, :].rearrange("p (c f) -> p c f", c=8),
                           [PB(tbank)], [("hnT", ii)])

                    def pre_r(ii):
                        for c in range(8):
                            mm(pb[6][:, ii * 20:(ii + 1) * 20], hnT[:, c, ii * 128:(ii + 1) * 128], wr[:, c, :], c == 0, c == 7,
                               [("hnT", ii), "wr"], [PB(6)])

                    for t in range(8 + 2):
                        if t < 8:
                            pre_n(t)
                        if 0 <= t - 1 < 8:
                            pre_t(t - 1)
                        if 0 <= t - 2 < 8:
                            pre_r(t - 2)
                    tt("dve", lg[:], pb[6][:, 0:160].rearrange("p (b f) -> p b f", f=20),
                       br_b[:].unsqueeze(1).to_broadcast([128, 8, 20]), ALU.add, [PB(6), "br_b"], ["lg"])
                    R = lambda *a: list(a)
                    S.op("dve", lambda e: e.tensor_reduce(gmx[:], lg[:, :, 0:4], mybir.AxisListType.X, ALU.max), ["lg"], ["gmx"])
                    tt("dve", oh[:], lg[:, :, 0:4], gmx[:].unsqueeze(2).to_broadcast([128, 8, 4]), ALU.is_ge, ["lg", "gmx"], ["oh"])
                    tt("dve", ge[:], lg[:, :, 0:4], gmx[:].unsqueeze(2).to_broadcast([128, 8, 4]), ALU.subtract, ["lg", "gmx"], ["ge"])
                    act(ge[:], ge[:], AF.Exp, ["ge"], ["ge"])
                    S.op("dve", lambda e: e.tensor_reduce(gs[:], ge[:], mybir.AxisListType.X, ALU.add), ["ge"], ["gs"])
                    rcp(gs[:], gs[:], ["gs"], ["gs"])
                    lge = lg[:, :, 4:20].rearrange("p b (g e) -> p b g e", g=4)
                    tt("dve", esel4[:], lge, oh[:].unsqueeze(3).to_broadcast([128, 8, 4, 4]), ALU.mult, ["lg", "oh"], ["esel4"])
                    S.op("dve", lambda e: e.tensor_reduce(esel[:], esel4[:].rearrange("p b g e -> p b e g"),
                                                          mybir.AxisListType.X, ALU.add), ["esel4"], ["esel"])
                    S.op("dve", lambda e: e.tensor_reduce(e1[:], esel[:], mybir.AxisListType.X, ALU.max), ["esel"], ["e1"])
                    tt("dve", sel[:], esel[:], e1[:].unsqueeze(2).to_broadcast([128, 8, 4]), ALU.is_ge, ["esel", "e1"], ["sel"])
                    stt("dve", em[:], sel[:], -1e30, esel[:], ALU.mult, ALU.add, ["sel", "esel"], ["em"])
                    S.op("dve", lambda e: e.tensor_reduce(e2[:], em[:], mybir.AxisListType.X, ALU.max), ["em"], ["e2"])
                    tt("dve", sel[:], esel[:], e2[:].unsqueeze(2).to_broadcast([128, 8, 4]), ALU.is_ge, ["esel", "e2"], ["sel"])
                    tt("dve", ew[:], esel[:], e1[:].unsqueeze(2).to_broadcast([128, 8, 4]), ALU.subtract, ["esel", "e1"], ["ew"])
                    act(ew[:], ew[:], AF.Exp, ["ew"], ["ew"])
                    tt("dve", ew[:], ew[:], sel[:], ALU.mult, ["ew", "sel"], ["ew"])
                    S.op("dve", lambda e: e.tensor_reduce(es[:], ew[:], mybir.AxisListType.X, ALU.add), ["ew"], ["es"])
                    rcp(es[:], es[:], ["es"], ["es"])
                    tt("dve", es[:], es[:], gs[:], ALU.mult, ["es", "gs"], ["es"])
                    tt("dve", ew[:], ew[:], es[:].unsqueeze(2).to_broadcast([128, 8, 4]), ALU.mult, ["ew", "es"], ["ew"])
                    tt("dve", cmb[:].rearrange("p b (g e) -> p b g e", g=4),
                       oh[:].unsqueeze(3).to_broadcast([128, 8, 4, 4]),
                       ew[:].unsqueeze(2).to_broadcast([128, 8, 4, 4]), ALU.mult, ["oh", "ew"], ["cmb"])

                    wei_v = w_ei.rearrange("e (c p) f -> e p c f", p=128)
                    weo_v = w_eo.rearrange("e (c p) f -> e p c f", p=128)
                    items = [(ex, ii) for ex in range(NE) for ii in range(8)]

                    def moe_a(t):
                        ex, ii = items[t]
                        wk = ex % 2
                        kk = t % 2
                        if ii == 0 and ex >= 2:
                            ldc(wei[:, wk], wei_v[ex], [("wei", wk)])
                            ldc(weo[:, wk], weo_v[ex], [("weo", wk)])
                        hb = kk
                        for c in range(8):
                            mm(pb[hb][:], hnT[:, c, ii * 128:(ii + 1) * 128], wei[:, wk, c, :], c == 0, c == 7,
                               [("hnT", ii), ("wei", wk)], [PB(hb)])
                        act(sg[:, kk, :], pb[hb][:, 0:256], AF.Silu, [PB(hb)], [("sg", kk)])
                        stt("dve", ac[:, kk, :], pb[hb][:, 256:512], cmb[:, ii, ex:ex + 1], sg[:, kk, :], ALU.mult, ALU.mult,
                            [PB(hb), "cmb", ("sg", kk)], [("ac", kk)])

                    def moe_b(t):
                        kk = t % 2
                        tb = 2 + kk
                        tp = pb[tb][:].bitcast(BF16)
                        tr(tp[:, 0:128], ac[:, kk, 0:128], [("ac", kk)], [PB(tb)])
                        tr(tp[:, 128:256], ac[:, kk, 128:256], [("ac", kk)], [PB(tb)])
                        cp("act", acT[:, kk, :], tp[:, 0:256], [PB(tb)], [("acT", kk)])

                    def moe_c(t):
                        ex, ii = items[t]
                        wk = ex % 2
                        kk = t % 2
                        for hf in range(2):
                            yb = 4 + 2 * kk + hf
                            for fc in range(2):
                                mm(pb[yb][:], acT[:, kk, fc * 128:(fc + 1) * 128], weo[:, wk, fc, hf * 512:(hf + 1) * 512],
                                   fc == 0, fc == 1, [("acT", kk), ("weo", wk)], [PB(yb)])
                            tt("dve", hres[:, ii, hf * 512:(hf + 1) * 512], pb[yb][:], hres[:, ii, hf * 512:(hf + 1) * 512],
                               ALU.add, [PB(yb), ("hres", ii, hf)], [("hres", ii, hf)])

                    NI = len(items)
                    for t in range(NI + 2):
                        if t < NI:
                            moe_a(t)
                        if 0 <= t - 1 < NI:
                            moe_b(t - 1)
                        if 0 <= t - 2 < NI:
                            moe_c(t - 2)
                    if stop == "E" and half == 0:
                        dump("hres", hres[:], [128, 8, D], F32, [("hres", b) for b in range(8)])
                        dump("cmb", cmb[:], [128, 8, 16], F32, ["cmb"])
                        return finish()
                    S.barrier()
                    S.flush()

                with ExitStack() as pst:
                    wpg = sb("wpg", [128, 8, D], BF16, pst)
                    wpp = sb("wpp", [128, 2, D], BF16, pst)
                    pTs = sb("pTs", [128, 2, 8 * 128], BF16, pst)
                    hn3 = sb("hn3", [128, 2, D], BF16, pst)
                    hn3T = sb("hn3T", [128, 2, D], BF16, pst)
                    junk3 = sb("junk3", [128, D], F32, pst)
                    ss3 = sb("ss3", [128, 8], F32, pst)
                    ss4 = sb("ss4", [128, 8], F32, pst)
                    th3 = sb("th3", [128, 2, D], F32, pst)
                    gple_b = sb("gple_b", [128, D], F32, pst)
                    gfin_b = sb("gfin_b", [128, D], F32, pst)
                    ld(gple_b[:], g_ple.partition_broadcast(128), ["gple_b"])
                    ld(gfin_b[:], g_fin.partition_broadcast(128), ["gfin_b"])
                    ldc(wpg[:], w_pg.rearrange("(c p) f -> p c f", p=128), ["wpg"])
                    ldc(wpp[:], w_pp.rearrange("(c p) f -> p c f", p=128), ["wpp"])
                    ldc(pTs[:], pT_own.rearrange("(c p) t -> p c t", p=128)[:, :, half * 1024:(half + 1) * 1024], ["pTs"])
                    ms("dve", ss3[:], EPS, [("ss3", ii_) for ii_ in range(8)])
                    ms("dve", ss4[:], EPS, [("ss4", ii_) for ii_ in range(8)])

                    def ple_n(ii):
                        kk = ii % 2
                        act(junk3[:], hres[:, ii, :], AF.Square, [("hres", ii)], ["junk3", ("ss3", ii)], accum=ss3[:, ii:ii + 1],
                            scale=float(D ** -0.5))
                        rstd(ss3[:, ii:ii + 1], ss3[:, ii:ii + 1], 1, [("ss3", ii)], [("ss3", ii)])
                        stt("dve", hn3[:, kk, :], hres[:, ii, :], ss3[:, ii:ii + 1], gple_b[:], ALU.mult, ALU.mult,
                            [("hres", ii), ("ss3", ii), "gple_b"], [("hn3", kk)])

                    def ple_t(ii):
                        kk = ii % 2
                        tbank = 0 + kk
                        tp = pb[tbank][:].bitcast(BF16)
                        for c in range(8):
                            tr(tp[:, c * 128:(c + 1) * 128], hn3[:, kk, c * 128:(c + 1) * 128], [("hn3", kk)], [PB(tbank)])
                        cp("act", hn3T[:, kk, :], tp[:, :], [PB(tbank)], [("hn3T", kk)])

                    def ple_g(ii):
                        i = half * 8 + ii
                        kk = ii % 2
                        for hf in range(2):
                            gbk = 2 + hf
                            for c in range(8):
                                mm(pb[gbk][:], hn3T[:, kk, c * 128:(c + 1) * 128], wpg[:, c, hf * 512:(hf + 1) * 512],
                                   c == 0, c == 7, [("hn3T", kk), "wpg"], [PB(gbk)])
                            act(th3[:, kk, hf * 512:(hf + 1) * 512], pb[gbk][:], AF.Tanh, [PB(gbk)], [("th3", kk, hf)], scale=0.5)
                            pbk = 4 + hf
                            for c in range(2):
                                mm(pb[pbk][:], pTs[:, c, ii * 128:(ii + 1) * 128], wpp[:, c, hf * 512:(hf + 1) * 512],
                                   c == 0, c == 1, ["pTs", "wpp"], [PB(pbk)])
                            th_ = th3[:, kk, hf * 512:(hf + 1) * 512]
                            stt("dve", th_, th_, 1.0, pb[pbk][:], ALU.add, ALU.mult, [("th3", kk, hf), PB(pbk)], [("th3", kk, hf)])
                            stt("dve", th_, th_, 0.5, hres[:, ii, hf * 512:(hf + 1) * 512], ALU.mult, ALU.add,
                                [("th3", kk, hf), ("hres", ii)], [("th3", kk, hf)])
                        act(junk3[:], th3[:, kk, :], AF.Square, [("th3", kk, 0), ("th3", kk, 1)], ["junk3", ("ss4", ii)],
                            accum=ss4[:, ii:ii + 1], scale=float(D ** -0.5))
                        rstd(ss4[:, ii:ii + 1], ss4[:, ii:ii + 1], 1, [("ss4", ii)], [("ss4", ii)])
                        stt("dve", th3[:, kk, :], th3[:, kk, :], ss4[:, ii:ii + 1], gfin_b[:], ALU.mult, ALU.mult,
                            [("th3", kk, 0), ("th3", kk, 1), ("ss4", ii), "gfin_b"], [("th3", kk, 0), ("th3", kk, 1)])
                        ld(out_own[i * 128:(i + 1) * 128, :], th3[:, kk, :], [("out", i)], r=[("th3", kk, 0), ("th3", kk, 1)])

                    for t in range(8 + 2):
                        if t < 8:
                            ple_n(t)
                        if 0 <= t - 1 < 8:
                            ple_t(t - 1)
                        if 0 <= t - 2 < 8:
                            ple_g(t - 2)
                    S.barrier()
                    S.flush()
    return nc


_NC_CACHE = {}


def _rope_tables():
    pos = np.arange(SEQ, dtype=np.float32)
    inv = (np.float32(10000.0) ** (-np.arange(0, 64, 2, dtype=np.float32) / np.float32(64))).astype(np.float32)
    ang = (pos[:, None] * inv[None, :]).astype(np.float32)
    return np.cos(ang).astype(np.float32), np.sin(ang).astype(np.float32)


def _prepare(x, p, g_mix, w_in, swa_sinks, mla_g_q, mla_w_uq, mla_g_kv, mla_w_ukv, w_out,
           g_ffn, w_router_group, b_router_group, w_router_expert, b_router_expert,
           w_expert_in, w_expert_out, g_ple, w_ple_gate, w_ple_proj, g_final):
    f = lambda a: np.ascontiguousarray(np.asarray(a, dtype=np.float32))
    x = f(x); p = f(p)
    B = x.shape[0]
    cos, sin = _rope_tables()
    cs_all = np.concatenate([cos, sin], axis=1)
    shared = {
        "cs_all": f(cs_all),
        "w_in": f(w_in[0]), "w_uq": f(mla_w_uq[0]), "w_ukv": f(mla_w_ukv[0]),
        "w_ukvT": f(np.asarray(mla_w_ukv[0]).T), "w_out": f(w_out[0]),
        "w_r": f(np.concatenate([np.asarray(w_router_group[0]), np.asarray(w_router_expert[0])], axis=1)),
        "b_r": f(np.concatenate([np.asarray(b_router_group[0]), np.asarray(b_router_expert[0])], axis=0)),
        "w_ei": f(w_expert_in[0]), "w_eo": f(w_expert_out[0]),
        "w_pg": f(w_ple_gate[0]), "w_pp": f(w_ple_proj[0]),
        "g_mixT": f(np.asarray(g_mix[0]).reshape(8, 128).T),
        "g_q": f(mla_g_q[0]), "g_kv": f(mla_g_kv[0]), "g_ffn": f(g_ffn[0]), "g_ple": f(g_ple[0]),
        "g_fin": f(g_final), "sinks": f(swa_sinks[0]),
    }
    kq = np.arange(128)
    tri_le = (kq[:, None] <= kq[None, :]).astype(np.float32)
    tri_gt = (kq[:, None] > kq[None, :]).astype(np.float32)
    in_maps = []
    own_rows = []
    for c in range(8):
        b, j = c // 4, c % 4
        blocks = np.array([4 * i + j for i in range(NOWN)])
        own_tok = (blocks[:, None] * 128 + np.arange(128)[None, :]).reshape(-1)
        prev_tok = own_tok.reshape(NOWN, 128) - 128
        pair_tok = np.concatenate([prev_tok, own_tok.reshape(NOWN, 128)], axis=1)
        valid = (pair_tok >= 0)
        pair_idx = np.where(valid, pair_tok, 0).reshape(-1)
        xb = x[b]
        x_pair = xb[pair_idx].copy()
        x_pair[~valid.reshape(-1)] = 0.0
        csT = np.zeros((128, 2, NOWN * 128), np.float32)
        csT[0:32, 0] = cos[own_tok].T; csT[32:64, 0] = cos[own_tok].T
        csT[0:32, 1] = sin[own_tok].T; csT[32:64, 1] = sin[own_tok].T
        cst = np.zeros((8, 128, 128), np.float32)
        cst[0] = np.eye(128, dtype=np.float32)
        for m in range(4):
            cst[1 + m] = 1.0 if m < j else (tri_le if m == j else 0.0)
        cst[5] = 0.0 if j == 0 else tri_gt
        cst[6] = tri_gt
        cst[7] = tri_le
        d = dict(shared)
        d.update({
            "xT_full": f(xb.T), "xT_q": f(xb[own_tok].T), "xT_pair": f(x_pair.T), "x_own": f(xb[own_tok]),
            "pT_own": f(p[0, b][own_tok].T), "csT_own": f(csT), "cs_pair": f(cs_all[pair_idx]),
            "consts": f(cst.transpose(1, 0, 2)),
        })
        in_maps.append(d)
        own_rows.append((b, own_tok))
    return in_maps, own_rows, B


def kernel(**inputs):
    in_maps, own_rows, B = _prepare(**inputs)
    if "nc" not in _NC_CACHE:
        _NC_CACHE["nc"] = build_program()
    res = run_bass_kernel_spmd(_NC_CACHE["nc"], in_maps, core_ids=list(range(8)))
    out = np.zeros((B, SEQ, D), np.float32)
    for c in range(8):
        b, own_tok = own_rows[c]
        out[b, own_tok] = np.asarray(res.results[c]["out_own"], dtype=np.float32)
    return out
```

```python
import numpy as np
import ml_dtypes
import concourse.bass as bass
import concourse.mybir as mybir
from concourse.bass_utils import run_bass_kernel_spmd

F32 = mybir.dt.float32
BF16 = mybir.dt.bfloat16
ALU = mybir.AluOpType
AF = mybir.ActivationFunctionType

D = 1024
SEQ = 8192
NB = 64
NOWN = 16
EPS = 1e-6
NE = 16
MLA_SCALE = 1.0 / np.sqrt(192.0)
SWA_SCALE = 1.0 / 8.0
ENG = ("pe", "act", "dve", "pool", "sp")
NSLOT = 24

DEBUG = False


class Sched:
    def __init__(self, nc, sems, dsems):
        self.nc = nc
        self.sems = sems
        self.dsems = dsems
        self.q = {e: [] for e in ENG}
        self.cnt = {e: 0 for e in ENG}
        self.seen = {e: {f: 0 for f in ENG} for e in ENG}
        self.seen_d = {e: [0] * NSLOT for e in ENG}
        self.last_w = {}
        self.readers = {}
        self.dma_q = {}
        self.slot_cnt = [0] * NSLOT

    def _waits(self, eng, reads, writes, extra=()):
        deps = list(extra)
        for k in reads:
            t = self.last_w.get(k)
            if t is not None:
                deps.append(t)
            if isinstance(k, tuple) and k[0] == "pb":
                deps.extend(r for r in self.readers.get(k, ()) if r[1] != eng)
        for k in writes:
            t = self.last_w.get(k)
            if t is not None:
                deps.append(t)
            deps.extend(self.readers.get(k, ()))
        waits = []
        for t in deps:
            if t[0] == "e":
                _, f, idx = t
                if f == eng and eng == "pe":
                    continue
                if self.seen[eng][f] >= idx:
                    continue
                self.seen[eng][f] = idx
                waits.append((self.sems[f], idx))
            else:
                _, slot, val = t
                if self.seen_d[eng][slot] >= val:
                    continue
                self.seen_d[eng][slot] = val
                waits.append((self.dsems[slot], val))
        return waits

    def _commit(self, tok, reads, writes):
        for k in writes:
            self.last_w[k] = tok
            self.readers[k] = []
        for k in reads:
            self.readers.setdefault(k, []).append(tok)

    def op(self, eng, fn, reads=(), writes=()):
        waits = self._waits(eng, reads, writes)
        self.cnt[eng] += 1
        tok = ("e", eng, self.cnt[eng])
        self.q[eng].append((waits, fn, (self.sems[eng], 1)))
        self._commit(tok, reads, writes)

    def dma(self, eng, fn, reads=(), writes=()):
        lo, n_ = (0, 16) if eng == "sp" else (16, NSLOT - 16)
        c = self.dma_q.get(eng, 0)
        self.dma_q[eng] = c + 1
        slot = lo + c % n_
        self.slot_cnt[slot] += 1
        val = 16 * self.slot_cnt[slot]
        extra = [("d", slot, val - 16)] if val > 16 else []
        waits = self._waits(eng, reads, writes, extra)
        tok = ("d", slot, val)
        self.q[eng].append((waits, fn, (self.dsems[slot], 16)))
        self._commit(tok, reads, writes)

    def barrier(self):
        for e in ENG:
            waits = []
            for f in ENG:
                if f != e and self.seen[e][f] < self.cnt[f]:
                    self.seen[e][f] = self.cnt[f]
                    waits.append((self.sems[f], self.cnt[f]))
            for s in range(NSLOT):
                n = self.slot_cnt[s]
                if n > 0 and self.seen_d[e][s] < 16 * n:
                    self.seen_d[e][s] = 16 * n
                    waits.append((self.dsems[s], 16 * n))
            if e != "pe":
                if self.seen[e][e] < self.cnt[e]:
                    self.seen[e][e] = self.cnt[e]
                    waits.append((self.sems[e], self.cnt[e]))
            self.q[e].append((waits, None, None))
        self.last_w = {}
        self.readers = {}

    def flush(self):
        nc = self.nc
        q = self.q

        def mk(name):
            def f(e):
                for waits, fn, inc in q[name]:
                    for sem, val in waits:
                        e.wait_ge(sem, val)
                    if fn is not None:
                        ins = fn(e)
                        ins.then_inc(inc[0], inc[1])
            return f

        with nc.Block() as blk:
            blk.tensor(mk("pe"))
            blk.scalar(mk("act"))
            blk.vector(mk("dve"))
            blk.gpsimd(mk("pool"))
            blk.sync(mk("sp"))
        self.q = {e: [] for e in ENG}


def build_program(stop=None):
    dbg = {}
    nc = bass.Bass("TRN2", target_bir_lowering=False)

    def din(name, shape):
        return nc.dram_tensor(name, list(shape), F32, kind="ExternalInput").ap()

    xT_full = din("xT_full", [D, SEQ])
    xT_q = din("xT_q", [D, NOWN * 128])
    xT_pair = din("xT_pair", [D, NOWN * 256])
    x_own = din("x_own", [NOWN * 128, D])
    pT_own = din("pT_own", [256, NOWN * 128])
    cs_all = din("cs_all", [SEQ, 64])
    csT_own = din("csT_own", [128, 2, NOWN * 128])
    cs_pair = din("cs_pair", [NOWN * 256, 64])
    consts = din("consts", [128, 8, 128])
    w_in = din("w_in", [D, 3776])
    w_uq = din("w_uq", [256, 1536])
    w_ukv = din("w_ukv", [128, 2048])
    w_ukvT = din("w_ukvT", [2048, 128])
    w_out = din("w_out", [D, D])
    w_r = din("w_r", [D, 20])
    b_r = din("b_r", [20])
    w_ei = din("w_ei", [NE, D, 512])
    w_eo = din("w_eo", [NE, 256, D])
    w_pg = din("w_pg", [D, D])
    w_pp = din("w_pp", [256, D])
    g_mixT = din("g_mixT", [128, 8])
    g_q = din("g_q", [256])
    g_kv = din("g_kv", [128])
    g_ffn = din("g_ffn", [D])
    g_ple = din("g_ple", [D])
    g_fin = din("g_fin", [D])
    sinks = din("sinks", [16])
    out_own = nc.dram_tensor("out_own", [NOWN * 128, D], F32, kind="ExternalOutput").ap()

    from contextlib import ExitStack

    with ExitStack() as top:
        uniq = [0]

        def sb(name, shape, dt, st=top):
            uniq[0] += 1
            return st.enter_context(nc.sbuf_tensor("%s_%d" % (name, uniq[0]), list(shape), dt))

        sems = {e: top.enter_context(nc.semaphore("s_" + e)) for e in ENG}
        dsems = [top.enter_context(nc.semaphore("d%d" % i)) for i in range(NSLOT)]
        S = Sched(nc, sems, dsems)
        pb = [top.enter_context(nc.psum_tensor("pb%d" % i, [128, 512], F32)) for i in range(8)]
        PB = lambda i: ("pb", i)

        def mm(out, lhsT, rhs, start, stop, r, w, skip=False):
            S.op("pe", lambda e: e.matmul(out, lhsT, rhs, start=start, stop=stop, skip_group_check=skip), r, w)

        def tr(out, in_, r, w):
            S.op("pe", lambda e: e.transpose(out, in_, ident), list(r) + ["consts"], w)

        def act(out, in_, func, r, w, scale=1.0, bias=None, accum=None):
            kw = {}
            if bias is not None:
                kw["bias"] = bias
            if accum is not None:
                kw["accum_out"] = accum
            S.op("act", lambda e: e.activation(out, in_, func, scale=scale, **kw), r, w)

        def tt(eng, out, a, b, op, r, w):
            S.op(eng, lambda e: e.tensor_tensor(out, a, b, op), r, w)

        def stt(eng, out, in0, scalar, in1, op0, op1, r, w):
            S.op(eng, lambda e: e.scalar_tensor_tensor(out, in0, scalar, in1, op0, op1), r, w)

        def ts(eng, out, in0, s1, s2, op0, op1, r, w):
            S.op(eng, lambda e: e.tensor_scalar(out, in0, s1, s2, op0, op1), r, w)

        def cp(eng, out, in_, r, w):
            if eng == "act":
                S.op("act", lambda e: e.copy(out, in_), r, w)
            else:
                S.op(eng, lambda e: e.tensor_copy(out, in_), r, w)

        def rcp(out, in_, r, w):
            S.op("dve", lambda e: e.reciprocal(out, in_), r, w)

        def rstd(out, in_, n, r, w):
            S.op("pool", lambda e: e.tensor_tensor(out, in_, expo[:, 0:n], ALU.pow), list(r) + ["expo"], w)

        def ms(eng, ap, val, w):
            S.op(eng, lambda e: e.memset(ap, val), (), w)

        def ld(out, in_, w, r=(), eng="sp"):
            S.dma(eng, lambda e: e.dma_start(out=out, in_=in_), r, w)

        def ldc(out, in_, w, r=()):
            S.dma("pool", lambda e: e.dma_start(out=out, in_=in_), r, w)

        def dump(name, ap, shape, dt, rkeys):
            t = nc.dram_tensor("dbg_" + name, list(shape), dt, kind="ExternalOutput").ap()
            ld(t, ap, [("dbg", name)], r=rkeys)

        def finish():
            S.barrier()
            S.flush()
            return nc

        cst = sb("cst", [128, 8, 128], BF16)
        ident = cst[:, 0, :]
        ones = sb("ones", [128, 128], BF16)
        epst = sb("epst", [128, 1], F32)
        gmix = sb("gmix", [128, 8], F32)
        esink = sb("esink", [128, 16], F32)
        o_b = sb("o_b", [128, NOWN, D], BF16)
        wB = sb("wB", [128, 8, 1280], BF16)
        wO = sb("wO", [128, 8, D], BF16)

        ldc(cst[:], consts, ["consts"])
        ms("dve", ones[:], 1.0 / 1024.0, ["ones"])
        ms("dve", epst[:], EPS, ["epst"])
        expo = sb("expo", [128, 512], F32)
        ms("pool", expo[:], -0.5, ["expo"])
        ld(gmix[:], g_mixT, ["gmix"])
        ld(esink[:], sinks.partition_broadcast(128), ["esink"])
        act(esink[:], esink[:], AF.Exp, ["esink"], ["esink"])

        def xn_keys(xn_key):
            return [(xn_key, c) for c in range(8)]

        with ExitStack() as kst:
            KT = sb("KT", [128, 2, SEQ], BF16, kst)
            Vg = sb("Vg", [128, NB, 129], BF16, kst)
            cqT = sb("cqT", [128, 2, NOWN * 128], BF16, kst)
            ms("pool", Vg[:, :, 128:129], 1.0, [("Vg", b) for b in range(NB)])
            wqn = sb("wqn", [128, 2, 8, 128], BF16, kst)
            wqr = sb("wqr", [128, 2, 8, 128], BF16, kst)
            wqt = sb("wqt", [128, 2, 8, 128], BF16, kst)
            wukT = sb("wukT", [128, 8, 128], BF16, kst)
            wuv = sb("wuv", [128, 8, 128], BF16, kst)

            with ExitStack() as ast:
                xb = sb("xb", [128, 3, 8, 512], BF16, ast)
                xsq = sb("xsq", [128, 8, 512], BF16, ast)
                csg = sb("csg", [128, 4, 4, 64], F32, ast)
                wkv = sb("wkv", [128, 8, 192], BF16, ast)
                wcq = sb("wcq", [128, 8, 256], BF16, ast)
                rsA = sb("rsA", [128, 4], F32, ast)
                r4 = sb("r4", [128, 2, 4], F32, ast)
                r2 = sb("r2", [128, 2, 4], F32, ast)
                ssy = sb("ssy", [128, 2, 4], F32, ast)
                sl = sb("sl", [128, 2, 4], F32, ast)
                junk = sb("junk", [128, 256], F32, ast)
                tA = sb("tA", [128, 2, 2, 32], F32, ast)
                tB = sb("tB", [128, 2, 2, 32], F32, ast)
                krt3 = sb("krt3", [128, 3, 128], BF16, ast)
                cqn = sb("cqn", [128, 2, 256], BF16, ast)
                gq_b = sb("gq_b", [128, 256], F32, ast)
                gkv_b = sb("gkv_b", [128, 128], F32, ast)
                ld(gq_b[:], g_q.partition_broadcast(128), ["gq_b"])
                ld(gkv_b[:], g_kv.partition_broadcast(128), ["gkv_b"])

                w_in_v = w_in.rearrange("(c p) f -> p c f", p=128)
                ldc(wkv[:], w_in_v[:, :, 1536:1728], ["wkv"])
                ldc(wcq[:], w_in_v[:, :, 1280:1536], ["wcq"])
                ms("dve", krt3[:], 0.0, [("krt", 0), ("krt", 1), ("krt", 2)])

                xTf = xT_full.rearrange("(c p) t -> p c t", p=128)
                csa = cs_all.rearrange("(b p) f -> p b f", p=128)
                xTq = xT_q.rearrange("(c p) t -> p c t", p=128)
                groups = [("kv", g) for g in range(NB // 4)] + [("cq", g) for g in range(NOWN // 4)]
                NG = len(groups)
                YB = ((1, 2), (5, 6))

                def g_load(gi):
                    kind, g = groups[gi]
                    k3 = gi % 3
                    if kind == "kv":
                        ldc(xb[:, k3], xTf[:, :, g * 512:(g + 1) * 512], [("xb", k3)])
                        ld(csg[:, gi % 4], csa[:, 4 * g:4 * g + 4, :], [("csg", gi % 4)])
                    else:
                        ldc(xb[:, k3], xTq[:, :, g * 512:(g + 1) * 512], [("xb", k3)])

                def g_sq(gi):
                    act(xsq[:], xb[:, gi % 3], AF.Square, [("xb", gi % 3)], ["xsq"])

                def g_stats(gi):
                    k = gi % 2
                    for b_ in range(4):
                        for c in range(8):
                            mm(pb[0][:, b_:b_ + 1], xsq[:, c, b_ * 128:(b_ + 1) * 128], ones[:, 0:1], b_ == 0 and c == 0, c == 7,
                               ["ones", "xsq"], [PB(0)], skip=True)
                    act(rsA[:, 0:4], pb[0][:, 0:4], AF.Identity, [PB(0), "epst"], ["rsA"], bias=epst[:, 0:1])
                    rstd(r4[:, k, :], rsA[:, 0:4], 4, ["rsA"], [("r4", k)])
                    tt("pool", r2[:, k, :], r4[:, k, :], r4[:, k, :], ALU.mult, [("r4", k)], [("r2", k)])

                def g_mm(gi):
                    kind, g = groups[gi]
                    k = gi % 2
                    k3 = gi % 3
                    w_, wkey, W, Wn = (wkv, "wkv", 192, 128) if kind == "kv" else (wcq, "wcq", 256, 256)
                    ms("dve", ssy[:, k, :], 0.0, [("ssy", k)])
                    for b_ in range(4):
                        bank = YB[k][b_ // 2]
                        off = (b_ % 2) * W
                        for c in range(8):
                            mm(pb[bank][:, off:off + W], xb[:, k3, c, b_ * 128:(b_ + 1) * 128], w_[:, c, :],
                               b_ % 2 == 0 and c == 0, c == 7, [("xb", k3), wkey], [PB(bank)], skip=True)
                    for b_ in range(4):
                        bank = YB[k][b_ // 2]
                        off = (b_ % 2) * W
                        act(junk[:, 0:Wn], pb[bank][:, off:off + Wn], AF.Square, [PB(bank)], ["junk", ("ssy", k)],
                            accum=ssy[:, k, b_:b_ + 1], scale=float(Wn ** -0.5))

                def g_sc_a(gi):
                    k = gi % 2
                    tt("dve", sl[:, k, :], ssy[:, k, :], r2[:, k, :], ALU.mult, [("ssy", k), ("r2", k)], [("sl", k)])
                    ts("dve", sl[:, k, :], sl[:, k, :], EPS, None, ALU.add, ALU.bypass, [("sl", k)], [("sl", k)])
                    rstd(sl[:, k, :], sl[:, k, :], 4, [("sl", k)], [("sl", k)])

                def g_sc_b(gi):
                    k = gi % 2
                    tt("dve", sl[:, k, :], sl[:, k, :], r4[:, k, :], ALU.mult, [("sl", k), ("r4", k)], [("sl", k)])

                def g_ev(gi, b_):
                    kind, g = groups[gi]
                    k = gi % 2
                    t = 4 * gi + b_
                    kk = t % 2
                    k3 = t % 3
                    blk = 4 * g + b_
                    bank = YB[k][b_ // 2]
                    if kind == "kv":
                        off = (b_ % 2) * 192
                        stt("dve", Vg[:, blk, 0:128], pb[bank][:, off:off + 128], sl[:, k, b_:b_ + 1], gkv_b[:],
                            ALU.mult, ALU.mult, [PB(bank), ("sl", k), "gkv_b"], [("Vg", blk)])
                        kr = pb[bank][:, off + 128:off + 192].rearrange("p (t f) -> p t f", t=2)
                        cosb = csg[:, gi % 4, b_, 0:32].unsqueeze(1).to_broadcast([128, 2, 32])
                        sinb = csg[:, gi % 4, b_, 32:64].unsqueeze(1).to_broadcast([128, 2, 32])
                        stt("dve", tA[:, kk], kr, r4[:, k, b_:b_ + 1], cosb, ALU.mult, ALU.mult,
                            [PB(bank), ("csg", gi % 4), ("r4", k)], [("tA", kk)])
                        stt("dve", tB[:, kk], kr, r4[:, k, b_:b_ + 1], sinb, ALU.mult, ALU.mult,
                            [PB(bank), ("csg", gi % 4), ("r4", k)], [("tB", kk)])
                        tt("dve", krt3[:, k3, 0:32], tA[:, kk, 0, :], tB[:, kk, 1, :], ALU.subtract,
                           [("tA", kk), ("tB", kk)], [("krt", k3)])
                        tt("dve", krt3[:, k3, 32:64], tA[:, kk, 1, :], tB[:, kk, 0, :], ALU.add,
                           [("tA", kk), ("tB", kk)], [("krt", k3)])
                    else:
                        off = (b_ % 2) * 256
                        stt("dve", cqn[:, kk, :], pb[bank][:, off:off + 256], sl[:, k, b_:b_ + 1], gq_b[:], ALU.mult, ALU.mult,
                            [PB(bank), ("sl", k), "gq_b"], [("cqn", kk)])

                def g_tr(gi, b_):
                    kind, g = groups[gi]
                    t = 4 * gi + b_
                    kk = t % 2
                    k3 = t % 3
                    blk = 4 * g + b_
                    tbank = 3 + kk
                    tp = pb[tbank][:].bitcast(BF16)
                    if kind == "kv":
                        tr(tp[:, 0:128], Vg[:, blk, 0:128], [("Vg", blk)], [PB(tbank)])
                        tr(tp[:, 128:256], krt3[:, k3, :], [("krt", k3)], [PB(tbank)])
                        cp("dve" if b_ % 2 == 0 else "act", KT[:, :, blk * 128:(blk + 1) * 128],
                           tp[:, 0:256].rearrange("p (t f) -> p t f", t=2), [PB(tbank)], [("KT", blk)])
                    else:
                        tr(tp[:, 0:128], cqn[:, kk, 0:128], [("cqn", kk)], [PB(tbank)])
                        tr(tp[:, 128:256], cqn[:, kk, 128:256], [("cqn", kk)], [PB(tbank)])
                        cp("dve" if b_ % 2 == 0 else "act", cqT[:, :, blk * 128:(blk + 1) * 128],
                           tp[:, 0:256].rearrange("p (t f) -> p t f", t=2), [PB(tbank)], [("cqT", blk)])

                g_load(0)
                g_load(1)
                g_load(2)
                wq_v = w_uq.rearrange("(c p) (h f) -> p c h f", p=128, f=192)
                for c in range(2):
                    ldc(wqn[:, c], wq_v[:, c, :, 0:128], ["wqn"])
                ms("pool", wqr[:], 0.0, ["wqr"])
                ms("pool", wqt[:], 0.0, ["wqt"])
                for c in range(2):
                    ldc(wqr[:, c, :, 0:64], wq_v[:, c, :, 128:192], ["wqr"])
                ldc(wukT[:], w_ukvT.rearrange("(h t n) r -> n h t r", t=2, n=128)[:, :, 0, :], ["wukT"])
                ldc(wuv[:], w_ukv.rearrange("r (h t v) -> r h t v", t=2, v=128)[:, :, 1, :], ["wuv"])
                for c in range(8):
                    ts("dve", wkv[:, c, :], wkv[:, c, :], gmix[:, c:c + 1], None, ALU.mult, ALU.bypass, ["wkv", "gmix"], ["wkv"])
                    ts("dve", wcq[:, c, :], wcq[:, c, :], gmix[:, c:c + 1], None, ALU.mult, ALU.bypass, ["wcq", "gmix"], ["wcq"])
                g_sq(0)
                g_stats(0)
                g_mm(0)
                g_sc_a(0)
                g_sq(1)
                g_sc_b(0)
                g_stats(1)
                for gi in range(NG):
                    if gi == 4:
                        ts("pool", wqt[:, :, :, 0:32], wqr[:, :, :, 32:64], -1.0, None, ALU.mult, ALU.bypass, ["wqr"], ["wqt"])
                        cp("pool", wqt[:, :, :, 32:64], wqr[:, :, :, 0:32], ["wqr"], ["wqt"])
                    if gi + 3 < NG:
                        g_load(gi + 3)
                    if gi + 2 < NG:
                        g_sq(gi + 2)
                    if gi + 1 < NG:
                        g_mm(gi + 1)
                    for b_ in range(5):
                        if b_ < 4:
                            g_ev(gi, b_)
                        if b_ >= 1:
                            g_tr(gi, b_ - 1)
                    if gi + 1 < NG:
                        g_sc_a(gi + 1)
                    if gi + 2 < NG:
                        g_stats(gi + 2)
                    if gi + 1 < NG:
                        g_sc_b(gi + 1)
                if stop == "A":
                    dump("KT", KT[:], [128, 2, SEQ], BF16, [("KT", b) for b in range(NB)])
                    dump("Vg", Vg[:], [128, NB, 129], BF16, [("Vg", b) for b in range(NB)])
                    dump("cqT", cqT[:], [128, 2, NOWN * 128], BF16, [("cqT", b) for b in range(NOWN)])
                    return finish()
                S.barrier()
                S.flush()

            with ExitStack() as mst:
                qabs = sb("qabs", [128, 2, 8, 512], BF16, mst)
                qrope = sb("qrope", [128, 2, 8, 512], BF16, mst)
                qn_sb = sb("qn_sb", [128, 2, 512], BF16, mst)
                csT = sb("csT", [128, 2, 2, 512], F32, mst)
                t1 = sb("t1", [128, 2, 512], F32, mst)
                t2 = sb("t2", [128, 2, 512], F32, mst)
                pT = sb("pT", [128, 4, 512], BF16, mst)
                rc = sb("rc", [128, 8], F32, mst)
                olat = sb("olat", [128, 2, D], BF16, mst)
                olT = sb("olT", [128, 2, D], BF16, mst)

                ldc(wB[:], w_in_v[:, :, 0:1280], ["wB"])
                ldc(wO[:], w_out.rearrange("(c p) f -> p c f", p=128), ["wO"])

                LOOK = 2

                def qprep(ig, h, part):
                    qk = ig % 2
                    cols = slice(ig * 512, (ig + 1) * 512)
                    hk2 = h % 2
                    cq_keys = [("cqT", 4 * ig + b_) for b_ in range(4)]
                    if part == 0:
                        if h == 0:
                            ld(csT[:, qk], csT_own[:, :, cols], [("csT", qk)])
                        for c in range(2):
                            mm(pb[6][:], wqn[:, c, h, :], cqT[:, c, cols], c == 0, c == 1, ["wqn"] + cq_keys, [PB(6)])
                        cp("dve", qn_sb[:, hk2, :], pb[6][:], [PB(6)], [("qn_sb", hk2)])
                    elif part == 1:
                        for c in range(2):
                            mm(pb[7][:], wqr[:, c, h, :], cqT[:, c, cols], c == 0, c == 1, ["wqr"] + cq_keys, [PB(7)])
                        tt("dve", t1[:, hk2, :], pb[7][:], csT[:, qk, 0, :], ALU.mult, [PB(7), ("csT", qk)], [("t1", hk2)])
                    elif part == 2:
                        mm(pb[6][:], wukT[:, h, :], qn_sb[:, hk2, :], True, True, ["wukT", ("qn_sb", hk2)], [PB(6)])
                        cp("dve", qabs[:, qk, h, :], pb[6][:], [PB(6)], [("qabs", qk, h)])
                    else:
                        for c in range(2):
                            mm(pb[7][:], wqt[:, c, h, :], cqT[:, c, cols], c == 0, c == 1, ["wqt"] + cq_keys, [PB(7)])
                        tt("dve", t2[:, hk2, :], pb[7][:], csT[:, qk, 1, :], ALU.mult, [PB(7), ("csT", qk)], [("t2", hk2)])
                        tt("pool", qrope[:, qk, h, :], t1[:, hk2, :], t2[:, hk2, :], ALU.add,
                           [("t1", hk2), ("t2", hk2)], [("qrope", qk, h)])

                def evac_a(i):
                    ok = i % 2
                    for bk in range(3):
                        nh = 3 if bk < 2 else 2
                        ov = pb[3 + bk][:, 0:nh * 129].rearrange("p (h f) -> p h f", f=129)
                        rcp(rc[:, 3 * bk:3 * bk + nh], ov[:, :, 128], [PB(3 + bk)], ["rc"])
                        tt("dve", olat[:, ok, 384 * bk:384 * bk + nh * 128].rearrange("p (h f) -> p h f", f=128),
                           ov[:, :, 0:128], rc[:, 3 * bk:3 * bk + nh].unsqueeze(2).to_broadcast([128, nh, 128]),
                           ALU.mult, [PB(3 + bk), "rc"], [("olat", ok)])

                def evac_b(i):
                    ok = i % 2
                    tp = pb[6][:].bitcast(BF16)
                    for h in range(8):
                        tr(tp[:, h * 128:(h + 1) * 128], olat[:, ok, h * 128:(h + 1) * 128], [("olat", ok)], [PB(6)])
                    cp("dve", olT[:, ok, :], tp[:, :], [PB(6)], [("olT", ok)])

                def evac_c(i, part):
                    ok = i % 2
                    for h in range(4 * part, 4 * part + 4):
                        mm(pb[7][:, (h % 4) * 128:(h % 4 + 1) * 128], olT[:, ok, h * 128:(h + 1) * 128], wuv[:, h, :],
                           True, True, [("olT", ok), "wuv"], [PB(7)])
                    cp("dve", o_b[:, i, 512 * part:512 * (part + 1)], pb[7][:], [PB(7)], [("o_b", i)])

                steps = []
                for i in range(NOWN):
                    for kb in range(4 * i + 4):
                        for hg in range(2):
                            steps.append((i, kb, hg))
                NS_ = len(steps)
                deferred = {}

                def defer(at, fn):
                    deferred.setdefault(min(at, NS_ - 1), []).append(fn)

                for h in range(8):
                    for part in range(4):
                        qprep(0, h, part)
                first_of_ig = {}
                for n, (i, kb, hg) in enumerate(steps):
                    if kb == 0 and hg == 0 and i % 4 == 0:
                        first_of_ig[i // 4] = n
                for ig in range(1, 4):
                    n0 = first_of_ig[ig - 1] + 4
                    for h in range(8):
                        for part in range(4):
                            defer(n0 + 2 * (4 * h + part), (lambda ig=ig, h=h, part=part: qprep(ig, h, part)))

                def s_stage(n):
                    i, kb, hg = steps[n]
                    qk = (i // 4) % 2
                    qc = slice((i % 4) * 128, (i % 4 + 1) * 128)
                    kcol = slice(kb * 128, (kb + 1) * 128)
                    sbk = n % 3
                    pk = n % 4
                    qkeys = [("qabs", qk, h) for h in range(4 * hg, 4 * hg + 4)]
                    rkeys = [("qrope", qk, h) for h in range(4 * hg, 4 * hg + 4)]
                    mm(pb[sbk][:].rearrange("p (h q) -> p h q", h=4), KT[:, 0, kcol], qabs[:, qk, 4 * hg:4 * hg + 4, qc], True, False,
                       [("KT", kb)] + qkeys, [PB(sbk)])
                    mm(pb[sbk][:].rearrange("p (h q) -> p h q", h=4), KT[:, 1, kcol], qrope[:, qk, 4 * hg:4 * hg + 4, qc], False, True,
                       [("KT", kb)] + rkeys, [PB(sbk)])
                    act(pT[:, pk, :], pb[sbk][:], AF.Exp, [PB(sbk)], [("pT", pk)], scale=float(MLA_SCALE))
                    if kb >= 4 * i:
                        m = kb - 4 * i
                        pv = pT[:, pk, :].rearrange("p (h q) -> p h q", h=4)
                        tt("dve", pv, pv, cst[:, 1 + m, :].unsqueeze(1).to_broadcast([128, 4, 128]), ALU.mult,
                           [("pT", pk), "consts"], [("pT", pk)])

                def p_stage(n):
                    i, kb, hg = steps[n]
                    nkb = 4 * i + 4
                    pk = n % 4
                    for hh in range(4):
                        h = 4 * hg + hh
                        ob = 3 + h // 3
                        oc = (h % 3) * 129
                        mm(pb[ob][:, oc:oc + 129], pT[:, pk, hh * 128:(hh + 1) * 128], Vg[:, kb, :],
                           kb == 0 and h % 3 == 0, kb == nkb - 1, [("pT", pk), ("Vg", kb)], [PB(ob)], skip=True)
                    if kb == nkb - 1 and hg == 1:
                        evac_a(i)
                        defer(n + 3, lambda i=i: evac_b(i))
                        defer(n + 5, lambda i=i: evac_c(i, 0))
                        defer(n + 7, lambda i=i: evac_c(i, 1))

                for n in range(NS_ + LOOK):
                    if n < NS_:
                        s_stage(n)
                    m_ = n - LOOK
                    if m_ >= 0:
                        p_stage(m_)
                        for fn in deferred.pop(m_, []):
                            fn()
                for k_ in sorted(deferred):
                    for fn in deferred[k_]:
                        fn()
                for c in range(8):
                    ts("dve", wB[:, c, :], wB[:, c, :], gmix[:, c:c + 1], None, ALU.mult, ALU.bypass, ["wB", "gmix"], ["wB"])
                if stop == "M":
                    dump("o_b", o_b[:], [128, NOWN, D], BF16, [("o_b", b) for b in range(NOWN)])
                    return finish()
                S.barrier()
                S.flush()

        wG = sb("wG", [128, 8, 2048], BF16)
        for half in range(2):
            with ExitStack() as hst:
                hres = sb("hres", [128, 8, D], F32, hst)
                with ExitStack() as gst:
                    xpb = sb("xpb", [128, 2, 8, 256], BF16, gst)
                    rr = sb("rr", [128, 2, 4], F32, gst)
                    csp = sb("csp", [128, 2, 2, 64], F32, gst)
                    xsq = sb("xsq2", [128, 8, 256], BF16, gst)
                    rs = sb("rs2", [128, 4], F32, gst)
                    qA = sb("qA", [128, D], F32, gst)
                    qB = sb("qB", [128, D], F32, gst)
                    qar = sb("qar", [128, D], BF16, gst)
                    kA = sb("kA", [128, 2, 128], F32, gst)
                    kB = sb("kB", [128, 2, 128], F32, gst)
                    kpad = sb("kpad", [128, 2, 4, 128], BF16, gst)
                    vaug = sb("vaug", [128, 2, 2, 65], BF16, gst)
                    qaT = sb("qaT", [128, 8, 128], BF16, gst)
                    kT = sb("kT", [128, 8, 128], BF16, gst)
                    pS = sb("pS", [128, 8, 512], BF16, gst)
                    den = sb("den", [128, 16], F32, gst)
                    oa = sb("oa", [128, 16, 64], F32, gst)
                    m1 = sb("m1", [128, D], BF16, gst)
                    m2 = sb("m2", [128, D], BF16, gst)
                    mg = sb("mg", [128, D], BF16, gst)
                    mgT = sb("mgT", [128, 8, 128], BF16, gst)

                    w_in_v = w_in.rearrange("(c p) f -> p c f", p=128)
                    ms("pool", kpad[:], 0.0, [("kpad", t_, p_) for t_ in range(2) for p_ in range(2)])
                    ms("pool", vaug[:, :, :, 64:65], 1.0, ["vaug"])
                    xTp = xT_pair.rearrange("(c p) t -> p c t", p=128)
                    csp_v = cs_pair.rearrange("(b p) f -> p b f", p=128)
                    th2 = sb("th2", [128, 2, 2048], BF16, gst)

                    def pg_load(ii):
                        i = half * 8 + ii
                        k = ii % 2
                        ldc(xpb[:, k], xTp[:, :, i * 256:(i + 1) * 256], [("xpb", k)])
                        ld(csp[:, k], csp_v[:, 2 * i:2 * i + 2, :], [("csp", k)])
                        ld(hres[:, ii, :], x_own[i * 128:(i + 1) * 128, :], [("hres", ii)])

                    def pg_sq(ii):
                        k = ii % 2
                        act(xsq[:], xpb[:, k], AF.Square, [("xpb", k)], ["xsq"])

                    def pg_x1a(ii):
                        k = ii % 2
                        for t in range(2):
                            for c in range(8):
                                mm(pb[1][:, t:t + 1], xsq[:, c, t * 128:(t + 1) * 128], ones[:, 0:1], t == 0 and c == 0, c == 7,
                                   ["ones", "xsq"], [PB(1)], skip=True)
                        act(rs[:, 0:2], pb[1][:, 0:2], AF.Identity, [PB(1), "epst"], ["rs"], bias=epst[:, 0:1])
                        rstd(rr[:, k, 0:2], rs[:, 0:2], 2, ["rs"], [("rr", k)])
                        ts("pool", rr[:, k, 2:3], rr[:, k, 1:2], 0.5, None, ALU.mult, ALU.bypass, [("rr", k)], [("rr", k)])
                        if ii + 1 < 8:
                            pg_load(ii + 1)

                    def pg_gate(ii, q4, parts=(0, 1), bank=None):
                        k = ii % 2
                        gb = 6 + (q4 % 2) if bank is None else bank
                        for part in parts:
                            for c in range(4 * part, 4 * part + 4):
                                mm(pb[gb][:], xpb[:, k, c, 128:256], wG[:, c, q4 * 512:(q4 + 1) * 512], c == 0, c == 7,
                                   [("xpb", k), ("wG", c)], [PB(gb)])
                            if part == 1:
                                act(th2[:, k, q4 * 512:(q4 + 1) * 512], pb[gb][:], AF.Tanh, [PB(gb), ("rr", k)], [("th", k, q4)],
                                    scale=rr[:, k, 2:3])

                    def pg_x2(ii):
                        k = ii % 2
                        for t in range(2):
                            for c in range(8):
                                mm(pb[5][:, t * 256:(t + 1) * 256], xpb[:, k, c, t * 128:(t + 1) * 128], wB[:, c, 1024:1280],
                                   c == 0, c == 7, [("xpb", k), "wB"], [PB(5)])
                        for hf in range(2):
                            for c in range(8):
                                mm(pb[6 + hf][:], xpb[:, k, c, 128:256], wB[:, c, hf * 512:(hf + 1) * 512], c == 0, c == 7,
                                   [("xpb", k), "wB"], [PB(6 + hf)])
                        m3 = lambda a: a.rearrange("p h t f -> p (h t) f")
                        for t in range(2):
                            kv = pb[5][:, t * 256:t * 256 + 128].rearrange("p (h t f) -> p h t f", h=2, t=2)
                            kAv = kA[:, t, :].rearrange("p (h t f) -> p h t f", h=2, t=2)
                            kBv = kB[:, t, :].rearrange("p (h t f) -> p h t f", h=2, t=2)
                            cos3 = csp[:, k, t, 0:32].unsqueeze(1).to_broadcast([128, 4, 32])
                            sin3 = csp[:, k, t, 32:64].unsqueeze(1).to_broadcast([128, 4, 32])
                            stt("dve", m3(kAv), m3(kv), rr[:, k, t:t + 1], cos3, ALU.mult, ALU.mult,
                                [PB(5), ("csp", k), ("rr", k)], [("kA", t)])
                            stt("dve", m3(kBv), m3(kv), rr[:, k, t:t + 1], sin3, ALU.mult, ALU.mult,
                                [PB(5), ("csp", k), ("rr", k)], [("kB", t)])
                            ts("dve", vaug[:, t, :, 0:64], pb[5][:, t * 256 + 128:(t + 1) * 256].rearrange("p (h f) -> p h f", h=2),
                               rr[:, k, t:t + 1], None, ALU.mult, ALU.bypass, [PB(5), ("rr", k)], ["vaug"])
                            kp5 = kpad[:, t].rearrange("p (hk par) d -> p hk par d", par=2)
                            for par in range(2):
                                kpv = kp5[:, :, par, par * 64:(par + 1) * 64].rearrange("p h (t f) -> p h t f", t=2)
                                tt("dve", kpv[:, :, 0, :], kAv[:, :, 0, :], kBv[:, :, 1, :], ALU.subtract,
                                   [("kA", t), ("kB", t)], [("kpad", t, par)])
                                tt("dve", kpv[:, :, 1, :], kAv[:, :, 1, :], kBv[:, :, 0, :], ALU.add,
                                   [("kA", t), ("kB", t)], [("kpad", t, par)])
                        cos3 = csp[:, k, 1, 0:32].unsqueeze(1).to_broadcast([128, 16, 32])
                        sin3 = csp[:, k, 1, 32:64].unsqueeze(1).to_broadcast([128, 16, 32])
                        for hf in range(2):
                            qv = pb[6 + hf][:].rearrange("p (h t f) -> p h t f", h=8, t=2)
                            qAv = qA[:, hf * 512:(hf + 1) * 512].rearrange("p (h t f) -> p h t f", h=8, t=2)
                            qBv = qB[:, hf * 512:(hf + 1) * 512].rearrange("p (h t f) -> p h t f", h=8, t=2)
                            stt("dve", m3(qAv), m3(qv), rr[:, k, 1:2], cos3, ALU.mult, ALU.mult,
                                [PB(6 + hf), ("csp", k), ("rr", k)], [("qA", hf)])
                            stt("dve", m3(qBv), m3(qv), rr[:, k, 1:2], sin3, ALU.mult, ALU.mult,
                                [PB(6 + hf), ("csp", k), ("rr", k)], [("qB", hf)])
                        qA4 = qA[:].rearrange("p (h t f) -> p h t f", h=16, t=2)
                        qB4 = qB[:].rearrange("p (h t f) -> p h t f", h=16, t=2)
                        qar4 = qar[:].rearrange("p (h t f) -> p h t f", h=16, t=2)
                        tt("dve", qar4[:, :, 0, :], qA4[:, :, 0, :], qB4[:, :, 1, :], ALU.subtract,
                           [("qA", 0), ("qA", 1), ("qB", 0), ("qB", 1)], ["qar"])
                        tt("dve", qar4[:, :, 1, :], qA4[:, :, 1, :], qB4[:, :, 0, :], ALU.add,
                           [("qA", 0), ("qA", 1), ("qB", 0), ("qB", 1)], ["qar"])

                    def pg_x3(ii, gates=False):
                        tp2 = pb[5][:].bitcast(BF16)
                        for t in range(2):
                            for v in range(4):
                                tr(tp2[:, (t * 4 + v) * 128:(t * 4 + v + 1) * 128], kpad[:, t, v, :], [("kpad", t, v % 2)], [PB(5)])
                        cp("dve", kT[:].rearrange("p c f -> p (c f)"), tp2[:, :], [PB(5)], ["kT"])
                        if gates:
                            pg_gate(ii, 2)
                        tp = pb[0][:].bitcast(BF16)
                        for c in range(8):
                            tr(tp[:, c * 128:(c + 1) * 128], qar[:, c * 128:(c + 1) * 128], ["qar"], [PB(0)])
                        cp("act", qaT[:].rearrange("p c f -> p (c f)"), tp[:, :], [PB(0)], ["qaT"])
                        if gates:
                            pg_gate(ii, 3)

                    def pg_y1(ii, nxt):
                        i = half * 8 + ii
                        sidx = 0
                        for hk in range(2):
                            for par in range(2):
                                for t in range(2):
                                    sbk = sidx % 2
                                    mm(pb[sbk][:].rearrange("p (h q) -> p h q", h=4), kT[:, t * 4 + hk * 2 + par, :],
                                       qaT[:, 4 * hk:4 * hk + 4, :], True, True, ["kT", "qaT"], [PB(sbk)])
                                    act(pS[:, sidx, :], pb[sbk][:], AF.Exp, [PB(sbk)], [("pS", sidx)], scale=float(SWA_SCALE))
                                    mi = 7 if t == 1 else (5 if i == 0 else 6)
                                    pv = pS[:, sidx, :].rearrange("p (h q) -> p h q", h=4)
                                    tt("dve", pv, pv,
                                       cst[:, mi, :].unsqueeze(1).to_broadcast([128, 4, 128]), ALU.mult,
                                       [("pS", sidx), "consts"], [("pS", sidx)])
                                    sidx += 1
                                    if nxt and sidx in (2, 4):
                                        pg_gate(ii + 1, 0, parts=(sidx // 2 - 1,))

                    def pg_y2(ii):
                        i = half * 8 + ii
                        k = ii % 2
                        started = set()
                        sidx = 0
                        for hk in range(2):
                            for par in range(2):
                                for t in range(2):
                                    for ci in range(4):
                                        head = 2 * (4 * hk + ci) + par
                                        ob = 2 + head // 7
                                        oc = (head % 7) * 65
                                        first = ob not in started
                                        started.add(ob)
                                        mm(pb[ob][:, oc:oc + 65], pS[:, sidx, ci * 128:(ci + 1) * 128], vaug[:, t, hk, :],
                                           first, t == 1, [("pS", sidx), "vaug"], [PB(ob)], skip=True)
                                    sidx += 1
                        for bk in range(3):
                            nh = 7 if bk < 2 else 2
                            ov = pb[2 + bk][:, 0:nh * 65].rearrange("p (h f) -> p h f", f=65)
                            tt("dve", den[:, 7 * bk:7 * bk + nh], ov[:, :, 64], esink[:, 7 * bk:7 * bk + nh], ALU.add,
                               [PB(2 + bk), "esink"], [("den", bk)])
                            rcp(den[:, 7 * bk:7 * bk + nh], den[:, 7 * bk:7 * bk + nh], [("den", bk)], [("den", bk)])
                            tt("dve", oa[:, 7 * bk:7 * bk + nh, :], ov[:, :, 0:64],
                               den[:, 7 * bk:7 * bk + nh].unsqueeze(2).to_broadcast([128, nh, 64]), ALU.mult,
                               [PB(2 + bk), ("den", bk)], [("oa", bk)])
                        oaf = oa[:].rearrange("p h f -> p (h f)")
                        stt("dve", m1[:], th2[:, k, 0:1024], 1.0, oaf, ALU.add, ALU.mult,
                            [("th", k, 0), ("th", k, 1), ("oa", 0), ("oa", 1), ("oa", 2)], ["m1"])
                        stt("dve", m2[:], th2[:, k, 1024:2048], 1.0, o_b[:, i, :], ALU.add, ALU.mult,
                            [("th", k, 2), ("th", k, 3), ("o_b", i)], ["m2"])
                        tt("dve", mg[:], m1[:], m2[:], ALU.add, ["m1", "m2"], ["mg"])

                    def pg_y3(ii, nxt=False):
                        if nxt:
                            pg_gate(ii + 1, 1, parts=(0,), bank=0)
                        tp = pb[1][:].bitcast(BF16)
                        for c in range(8):
                            tr(tp[:, c * 128:(c + 1) * 128], mg[:, c * 128:(c + 1) * 128], ["mg"], [PB(1)])
                        cp("act", mgT[:].rearrange("p c f -> p (c f)"), tp[:, :], [PB(1)], ["mgT"])
                        if nxt:
                            pg_gate(ii + 1, 1, parts=(1,), bank=0)

                    def pg_y4(ii):
                        for hf in range(2):
                            for c in range(8):
                                mm(pb[2 + hf][:], mgT[:, c, :], wO[:, c, hf * 512:(hf + 1) * 512], c == 0, c == 7,
                                   ["mgT", "wO"], [PB(2 + hf)])
                            stt("dve", hres[:, ii, hf * 512:(hf + 1) * 512], pb[2 + hf][:], 0.5,
                                hres[:, ii, hf * 512:(hf + 1) * 512], ALU.mult, ALU.add,
                                [PB(2 + hf), ("hres", ii)], [("hres", ii)])

                    pg_load(0)
                    if half == 0:
                        for c in range(8):
                            ldc(wG[:, c, :], w_in_v[:, c, 1728:3776], [("wG", c)])
                    pg_sq(0)
                    pg_x1a(0)
                    pg_x2(0)
                    pg_x3(0)
                    if half == 0:
                        for c in range(8):
                            ts("dve", wG[:, c, :], wG[:, c, :], gmix[:, c:c + 1], None, ALU.mult, ALU.bypass,
                               [("wG", c), "gmix"], [("wG", c)])
                    for q4 in range(4):
                        pg_gate(0, q4)
                    pg_sq(1)
                    for ii in range(8):
                        nxt = ii + 1 < 8
                        if nxt:
                            pg_x1a(ii + 1)
                        pg_y1(ii, nxt)
                        pg_y2(ii)
                        if ii + 2 < 8:
                            pg_sq(ii + 2)
                        if nxt:
                            pg_x2(ii + 1)
                        pg_y3(ii, nxt)
                        pg_y4(ii)
                        if nxt:
                            pg_x3(ii + 1, gates=True)
                    if stop == "G" and half == 0:
                        dump("hres", hres[:], [128, 8, D], F32, [("hres", b) for b in range(8)])
                        return finish()
                    S.barrier()
                    S.flush()

                with ExitStack() as est:
                    hnT = sb("hnT", [128, 8, 8 * 128], BF16, est)
                    hn = sb("hn", [128, 2, D], BF16, est)
                    junk2 = sb("junk2", [128, D], F32, est)
                    ssh = sb("ssh", [128, 8], F32, est)
                    wr = sb("wr", [128, 8, 20], BF16, est)
                    lg = sb("lg", [128, 8, 20], F32, est)
                    gmx = sb("gmx", [128, 8], F32, est)
                    oh = sb("oh", [128, 8, 4], F32, est)
                    ge = sb("ge", [128, 8, 4], F32, est)
                    gs = sb("gs", [128, 8], F32, est)
                    esel4 = sb("esel4", [128, 8, 4, 4], F32, est)
                    esel = sb("esel", [128, 8, 4], F32, est)
                    e1 = sb("e1", [128, 8], F32, est)
                    em = sb("em", [128, 8, 4], F32, est)
                    e2 = sb("e2", [128, 8], F32, est)
                    sel = sb("sel", [128, 8, 4], F32, est)
                    ew = sb("ew", [128, 8, 4], F32, est)
                    es = sb("es", [128, 8], F32, est)
                    cmb = sb("cmb", [128, 8, 16], F32, est)
                    wei = sb("wei", [128, 2, 8, 512], BF16, est)
                    weo = sb("weo", [128, 2, 2, D], BF16, est)
                    sg = sb("sg", [128, 2, 256], F32, est)
                    ac = sb("ac", [128, 2, 256], BF16, est)
                    acT = sb("acT", [128, 2, 256], BF16, est)
                    gffn_b = sb("gffn_b", [128, D], F32, est)
                    br_b = sb("br_b", [128, 20], F32, est)
                    ld(gffn_b[:], g_ffn.partition_broadcast(128), ["gffn_b"])
                    ld(br_b[:], b_r.partition_broadcast(128), ["br_b"])

                    ldc(wr[:], w_r.rearrange("(c p) f -> p c f", p=128), ["wr"])
                    wei_v = w_ei.rearrange("e (c p) f -> e p c f", p=128)
                    weo_v = w_eo.rearrange("e (c p) f -> e p c f", p=128)
                    for ex_ in range(2):
                        ldc(wei[:, ex_], wei_v[ex_], [("wei", ex_)])
                        ldc(weo[:, ex_], weo_v[ex_], [("weo", ex_)])
                    ms("dve", ssh[:], EPS, [("ssh", ii_) for ii_ in range(8)])

                    def pre_n(ii):
                        kk = ii % 2
                        act(junk2[:], hres[:, ii, :], AF.Square, [("hres", ii)], ["junk2", ("ssh", ii)], accum=ssh[:, ii:ii + 1],
                            scale=float(D ** -0.5))
                        rstd(ssh[:, ii:ii + 1], ssh[:, ii:ii + 1], 1, [("ssh", ii)], [("ssh", ii)])
                        stt("dve", hn[:, kk, :], hres[:, ii, :], ssh[:, ii:ii + 1], gffn_b[:], ALU.mult, ALU.mult,
                            [("hres", ii), ("ssh", ii), "gffn_b"], [("hn", kk)])

                    def pre_t(ii):
                        kk = ii % 2
                        tbank = 4 + kk
                        tp = pb[tbank][:].bitcast(BF16)
                        for c in range(8):
                            tr(tp[:, c * 128:(c + 1) * 128], hn[:, kk, c * 128:(c + 1) * 128], [("hn", kk)], [PB(tbank)])
                        cp("act", hnT[:, :, ii * 128:(ii + 1) * 128], tp[:, :].rearrange("p (c f) -> p c f", c=8),
                           [PB(tbank)], [("hnT", ii)])

                    def pre_r(ii):
                        for c in range(8):
                            mm(pb[6][:, ii * 20:(ii + 1) * 20], hnT[:, c, ii * 128:(ii + 1) * 128], wr[:, c, :], c == 0, c == 7,
                               [("hnT", ii), "wr"], [PB(6)])

                    for t in range(8 + 2):
                        if t < 8:
                            pre_n(t)
                        if 0 <= t - 1 < 8:
                            pre_t(t - 1)
                        if 0 <= t - 2 < 8:
                            pre_r(t - 2)
                    tt("dve", lg[:], pb[6][:, 0:160].rearrange("p (b f) -> p b f", f=20),
                       br_b[:].unsqueeze(1).to_broadcast([128, 8, 20]), ALU.add, [PB(6), "br_b"], ["lg"])
                    R = lambda *a: list(a)
                    S.op("dve", lambda e: e.tensor_reduce(gmx[:], lg[:, :, 0:4], mybir.AxisListType.X, ALU.max), ["lg"], ["gmx"])
                    tt("dve", oh[:], lg[:, :, 0:4], gmx[:].unsqueeze(2).to_broadcast([128, 8, 4]), ALU.is_ge, ["lg", "gmx"], ["oh"])
                    tt("dve", ge[:], lg[:, :, 0:4], gmx[:].unsqueeze(2).to_broadcast([128, 8, 4]), ALU.subtract, ["lg", "gmx"], ["ge"])
                    act(ge[:], ge[:], AF.Exp, ["ge"], ["ge"])
                    S.op("dve", lambda e: e.tensor_reduce(gs[:], ge[:], mybir.AxisListType.X, ALU.add), ["ge"], ["gs"])
                    rcp(gs[:], gs[:], ["gs"], ["gs"])
                    lge = lg[:, :, 4:20].rearrange("p b (g e) -> p b g e", g=4)
                    tt("dve", esel4[:], lge, oh[:].unsqueeze(3).to_broadcast([128, 8, 4, 4]), ALU.mult, ["lg", "oh"], ["esel4"])
                    S.op("dve", lambda e: e.tensor_reduce(esel[:], esel4[:].rearrange("p b g e -> p b e g"),
                                                          mybir.AxisListType.X, ALU.add), ["esel4"], ["esel"])
                    S.op("dve", lambda e: e.tensor_reduce(e1[:], esel[:], mybir.AxisListType.X, ALU.max), ["esel"], ["e1"])
                    tt("dve", sel[:], esel[:], e1[:].unsqueeze(2).to_broadcast([128, 8, 4]), ALU.is_ge, ["esel", "e1"], ["sel"])
                    stt("dve", em[:], sel[:], -1e30, esel[:], ALU.mult, ALU.add, ["sel", "esel"], ["em"])
                    S.op("dve", lambda e: e.tensor_reduce(e2[:], em[:], mybir.AxisListType.X, ALU.max), ["em"], ["e2"])
                    tt("dve", sel[:], esel[:], e2[:].unsqueeze(2).to_broadcast([128, 8, 4]), ALU.is_ge, ["esel", "e2"], ["sel"])
                    tt("dve", ew[:], esel[:], e1[:].unsqueeze(2).to_broadcast([128, 8, 4]), ALU.subtract, ["esel", "e1"], ["ew"])
                    act(ew[:], ew[:], AF.Exp, ["ew"], ["ew"])
                    tt("dve", ew[:], ew[:], sel[:], ALU.mult, ["ew", "sel"], ["ew"])
                    S.op("dve", lambda e: e.tensor_reduce(es[:], ew[:], mybir.AxisListType.X, ALU.add), ["ew"], ["es"])
                    rcp(es[:], es[:], ["es"], ["es"])
                    tt("dve", es[:], es[:], gs[:], ALU.mult, ["es", "gs"], ["es"])
                    tt("dve", ew[:], ew[:], es[:].unsqueeze(2).to_broadcast([128, 8, 4]), ALU.mult, ["ew", "es"], ["ew"])
                    tt("dve", cmb[:].rearrange("p b (g e) -> p b g e", g=4),
                       oh[:].unsqueeze(3).to_broadcast([128, 8, 4, 4]),
                       ew[:].unsqueeze(2).to_broadcast([128, 8, 4, 4]), ALU.mult, ["oh", "ew"], ["cmb"])

                    wei_v = w_ei.rearrange("e (c p) f -> e p c f", p=128)
                    weo_v = w_eo.rearrange("e (c p) f -> e p c f", p=128)
                    items = [(ex, ii) for ex in range(NE) for ii in range(8)]

                    def moe_a(t):
                        ex, ii = items[t]
                        wk = ex % 2
                        kk = t % 2
                        if ii == 0 and ex >= 2:
                            ldc(wei[:, wk], wei_v[ex], [("wei", wk)])
                            ldc(weo[:, wk], weo_v[ex], [("weo", wk)])
                        hb = kk
                        for c in range(8):
                            mm(pb[hb][:], hnT[:, c, ii * 128:(ii + 1) * 128], wei[:, wk, c, :], c == 0, c == 7,
                               [("hnT", ii), ("wei", wk)], [PB(hb)])
                        act(sg[:, kk, :], pb[hb][:, 0:256], AF.Silu, [PB(hb)], [("sg", kk)])
                        stt("dve", ac[:, kk, :], pb[hb][:, 256:512], cmb[:, ii, ex:ex + 1], sg[:, kk, :], ALU.mult, ALU.mult,
                            [PB(hb), "cmb", ("sg", kk)], [("ac", kk)])

                    def moe_b(t):
                        kk = t % 2
                        tb = 2 + kk
                        tp = pb[tb][:].bitcast(BF16)
                        tr(tp[:, 0:128], ac[:, kk, 0:128], [("ac", kk)], [PB(tb)])
                        tr(tp[:, 128:256], ac[:, kk, 128:256], [("ac", kk)], [PB(tb)])
                        cp("act", acT[:, kk, :], tp[:, 0:256], [PB(tb)], [("acT", kk)])

                    def moe_c(t):
                        ex, ii = items[t]
                        wk = ex % 2
                        kk = t % 2
                        for hf in range(2):
                            yb = 4 + 2 * kk + hf
                            for fc in range(2):
                                mm(pb[yb][:], acT[:, kk, fc * 128:(fc + 1) * 128], weo[:, wk, fc, hf * 512:(hf + 1) * 512],
                                   fc == 0, fc == 1, [("acT", kk), ("weo", wk)], [PB(yb)])
                            tt("dve", hres[:, ii, hf * 512:(hf + 1) * 512], pb[yb][:], hres[:, ii, hf * 512:(hf + 1) * 512],
                               ALU.add, [PB(yb), ("hres", ii, hf)], [("hres", ii, hf)])

                    NI = len(items)
                    for t in range(NI + 2):
                        if t < NI:
                            moe_a(t)
                        if 0 <= t - 1 < NI:
                            moe_b(t - 1)
                        if 0 <= t - 2 < NI:
                            moe_c(t - 2)
                    if stop == "E" and half == 0:
                        dump("hres", hres[:], [128, 8, D], F32, [("hres", b) for b in range(8)])
                        dump("cmb", cmb[:], [128, 8, 16], F32, ["cmb"])
                        return finish()
                    S.barrier()
                    S.flush()

                with ExitStack() as pst:
                    wpg = sb("wpg", [128, 8, D], BF16, pst)
                    wpp = sb("wpp", [128, 2, D], BF16, pst)
                    pTs = sb("pTs", [128, 2, 8 * 128], BF16, pst)
                    hn3 = sb("hn3", [128, 2, D], BF16, pst)
                    hn3T = sb("hn3T", [128, 2, D], BF16, pst)
                    junk3 = sb("junk3", [128, D], F32, pst)
                    ss3 = sb("ss3", [128, 8], F32, pst)
                    ss4 = sb("ss4", [128, 8], F32, pst)
                    th3 = sb("th3", [128, 2, D], F32, pst)
                    gple_b = sb("gple_b", [128, D], F32, pst)
                    gfin_b = sb("gfin_b", [128, D], F32, pst)
                    ld(gple_b[:], g_ple.partition_broadcast(128), ["gple_b"])
                    ld(gfin_b[:], g_fin.partition_broadcast(128), ["gfin_b"])
                    ldc(wpg[:], w_pg.rearrange("(c p) f -> p c f", p=128), ["wpg"])
                    ldc(wpp[:], w_pp.rearrange("(c p) f -> p c f", p=128), ["wpp"])
                    ldc(pTs[:], pT_own.rearrange("(c p) t -> p c t", p=128)[:, :, half * 1024:(half + 1) * 1024], ["pTs"])
                    ms("dve", ss3[:], EPS, [("ss3", ii_) for ii_ in range(8)])
                    ms("dve", ss4[:], EPS, [("ss4", ii_) for ii_ in range(8)])

                    def ple_n(ii):
                        kk = ii % 2
                        act(junk3[:], hres[:, ii, :], AF.Square, [("hres", ii)], ["junk3", ("ss3", ii)], accum=ss3[:, ii:ii + 1],
                            scale=float(D ** -0.5))
                        rstd(ss3[:, ii:ii + 1], ss3[:, ii:ii + 1], 1, [("ss3", ii)], [("ss3", ii)])
                        stt("dve", hn3[:, kk, :], hres[:, ii, :], ss3[:, ii:ii + 1], gple_b[:], ALU.mult, ALU.mult,
                            [("hres", ii), ("ss3", ii), "gple_b"], [("hn3", kk)])

                    def ple_t(ii):
                        kk = ii % 2
                        tbank = 0 + kk
                        tp = pb[tbank][:].bitcast(BF16)
                        for c in range(8):
                            tr(tp[:, c * 128:(c + 1) * 128], hn3[:, kk, c * 128:(c + 1) * 128], [("hn3", kk)], [PB(tbank)])
                        cp("act", hn3T[:, kk, :], tp[:, :], [PB(tbank)], [("hn3T", kk)])

                    def ple_g(ii):
                        i = half * 8 + ii
                        kk = ii % 2
                        for hf in range(2):
                            gbk = 2 + hf
                            for c in range(8):
                                mm(pb[gbk][:], hn3T[:, kk, c * 128:(c + 1) * 128], wpg[:, c, hf * 512:(hf + 1) * 512],
                                   c == 0, c == 7, [("hn3T", kk), "wpg"], [PB(gbk)])
                            act(th3[:, kk, hf * 512:(hf + 1) * 512], pb[gbk][:], AF.Tanh, [PB(gbk)], [("th3", kk, hf)], scale=0.5)
                            pbk = 4 + hf
                            for c in range(2):
                                mm(pb[pbk][:], pTs[:, c, ii * 128:(ii + 1) * 128], wpp[:, c, hf * 512:(hf + 1) * 512],
                                   c == 0, c == 1, ["pTs", "wpp"], [PB(pbk)])
                            th_ = th3[:, kk, hf * 512:(hf + 1) * 512]
                            stt("dve", th_, th_, 1.0, pb[pbk][:], ALU.add, ALU.mult, [("th3", kk, hf), PB(pbk)], [("th3", kk, hf)])
                            stt("dve", th_, th_, 0.5, hres[:, ii, hf * 512:(hf + 1) * 512], ALU.mult, ALU.add,
                                [("th3", kk, hf), ("hres", ii)], [("th3", kk, hf)])
                        act(junk3[:], th3[:, kk, :], AF.Square, [("th3", kk, 0), ("th3", kk, 1)], ["junk3", ("ss4", ii)],
                            accum=ss4[:, ii:ii + 1], scale=float(D ** -0.5))
                        rstd(ss4[:, ii:ii + 1], ss4[:, ii:ii + 1], 1, [("ss4", ii)], [("ss4", ii)])
                        stt("dve", th3[:, kk, :], th3[:, kk, :], ss4[:, ii:ii + 1], gfin_b[:], ALU.mult, ALU.mult,
                            [("th3", kk, 0), ("th3", kk, 1), ("ss4", ii), "gfin_b"], [("th3", kk, 0), ("th3", kk, 1)])
                        ld(out_own[i * 128:(i + 1) * 128, :], th3[:, kk, :], [("out", i)], r=[("th3", kk, 0), ("th3", kk, 1)])

                    for t in range(8 + 2):
                        if t < 8:
                            ple_n(t)
                        if 0 <= t - 1 < 8:
                            ple_t(t - 1)
                        if 0 <= t - 2 < 8:
                            ple_g(t - 2)
                    S.barrier()
                    S.flush()
    return nc


_NC_CACHE = {}


def _rope_tables():
    pos = np.arange(SEQ, dtype=np.float32)
    inv = (np.float32(10000.0) ** (-np.arange(0, 64, 2, dtype=np.float32) / np.float32(64))).astype(np.float32)
    ang = (pos[:, None] * inv[None, :]).astype(np.float32)
    return np.cos(ang).astype(np.float32), np.sin(ang).astype(np.float32)


def _prepare(x, p, g_mix, w_in, swa_sinks, mla_g_q, mla_w_uq, mla_g_kv, mla_w_ukv, w_out,
           g_ffn, w_router_group, b_router_group, w_router_expert, b_router_expert,
           w_expert_in, w_expert_out, g_ple, w_ple_gate, w_ple_proj, g_final):
    f = lambda a: np.ascontiguousarray(np.asarray(a, dtype=np.float32))
    x = f(x); p = f(p)
    B = x.shape[0]
    cos, sin = _rope_tables()
    cs_all = np.concatenate([cos, sin], axis=1)
    shared = {
        "cs_all": f(cs_all),
        "w_in": f(w_in[0]), "w_uq": f(mla_w_uq[0]), "w_ukv": f(mla_w_ukv[0]),
        "w_ukvT": f(np.asarray(mla_w_ukv[0]).T), "w_out": f(w_out[0]),
        "w_r": f(np.concatenate([np.asarray(w_router_group[0]), np.asarray(w_router_expert[0])], axis=1)),
        "b_r": f(np.concatenate([np.asarray(b_router_group[0]), np.asarray(b_router_expert[0])], axis=0)),
        "w_ei": f(w_expert_in[0]), "w_eo": f(w_expert_out[0]),
        "w_pg": f(w_ple_gate[0]), "w_pp": f(w_ple_proj[0]),
        "g_mixT": f(np.asarray(g_mix[0]).reshape(8, 128).T),
        "g_q": f(mla_g_q[0]), "g_kv": f(mla_g_kv[0]), "g_ffn": f(g_ffn[0]), "g_ple": f(g_ple[0]),
        "g_fin": f(g_final), "sinks": f(swa_sinks[0]),
    }
    kq = np.arange(128)
    tri_le = (kq[:, None] <= kq[None, :]).astype(np.float32)
    tri_gt = (kq[:, None] > kq[None, :]).astype(np.float32)
    in_maps = []
    own_rows = []
    for c in range(8):
        b, j = c // 4, c % 4
        blocks = np.array([4 * i + j for i in range(NOWN)])
        own_tok = (blocks[:, None] * 128 + np.arange(128)[None, :]).reshape(-1)
        prev_tok = own_tok.reshape(NOWN, 128) - 128
        pair_tok = np.concatenate([prev_tok, own_tok.reshape(NOWN, 128)], axis=1)
        valid = (pair_tok >= 0)
        pair_idx = np.where(valid, pair_tok, 0).reshape(-1)
        xb = x[b]
        x_pair = xb[pair_idx].copy()
        x_pair[~valid.reshape(-1)] = 0.0
        csT = np.zeros((128, 2, NOWN * 128), np.float32)
        csT[0:32, 0] = cos[own_tok].T; csT[32:64, 0] = cos[own_tok].T
        csT[0:32, 1] = sin[own_tok].T; csT[32:64, 1] = sin[own_tok].T
        cst = np.zeros((8, 128, 128), np.float32)
        cst[0] = np.eye(128, dtype=np.float32)
        for m in range(4):
            cst[1 + m] = 1.0 if m < j else (tri_le if m == j else 0.0)
        cst[5] = 0.0 if j == 0 else tri_gt
        cst[6] = tri_gt
        cst[7] = tri_le
        d = dict(shared)
        d.update({
            "xT_full": f(xb.T), "xT_q": f(xb[own_tok].T), "xT_pair": f(x_pair.T), "x_own": f(xb[own_tok]),
            "pT_own": f(p[0, b][own_tok].T), "csT_own": f(csT), "cs_pair": f(cs_all[pair_idx]),
            "consts": f(cst.transpose(1, 0, 2)),
        })
        in_maps.append(d)
        own_rows.append((b, own_tok))
    return in_maps, own_rows, B


def kernel(**inputs):
    in_maps, own_rows, B = _prepare(**inputs)
    if "nc" not in _NC_CACHE:
        _NC_CACHE["nc"] = build_program()
    res = run_bass_kernel_spmd(_NC_CACHE["nc"], in_maps, core_ids=list(range(8)))
    out = np.zeros((B, SEQ, D), np.float32)
    for c in range(8):
        b, own_tok = own_rows[c]
        out[b, own_tok] = np.asarray(res.results[c]["out_own"], dtype=np.float32)
    return out
```

```python
import numpy as np
import ml_dtypes
import concourse.bass as bass
import concourse.mybir as mybir
from concourse.bass_utils import run_bass_kernel_spmd

F32 = mybir.dt.float32
BF16 = mybir.dt.bfloat16
ALU = mybir.AluOpType
AF = mybir.ActivationFunctionType

D = 1024
SEQ = 8192
NB = 64
NOWN = 16
EPS = 1e-6
NE = 16
MLA_SCALE = 1.0 / np.sqrt(192.0)
SWA_SCALE = 1.0 / 8.0
ENG = ("pe", "act", "dve", "pool", "sp")
NSLOT = 24

DEBUG = False


class Sched:
    def __init__(self, nc, sems, dsems):
        self.nc = nc
        self.sems = sems
        self.dsems = dsems
        self.q = {e: [] for e in ENG}
        self.cnt = {e: 0 for e in ENG}
        self.seen = {e: {f: 0 for f in ENG} for e in ENG}
        self.seen_d = {e: [0] * NSLOT for e in ENG}
        self.last_w = {}
        self.readers = {}
        self.dma_q = {}
        self.slot_cnt = [0] * NSLOT

    def _waits(self, eng, reads, writes, extra=()):
        deps = list(extra)
        for k in reads:
            t = self.last_w.get(k)
            if t is not None:
                deps.append(t)
            if isinstance(k, tuple) and k[0] == "pb":
                deps.extend(r for r in self.readers.get(k, ()) if r[1] != eng)
        for k in writes:
            t = self.last_w.get(k)
            if t is not None:
                deps.append(t)
            deps.extend(self.readers.get(k, ()))
        waits = []
        for t in deps:
            if t[0] == "e":
                _, f, idx = t
                if f == eng and eng == "pe":
                    continue
                if self.seen[eng][f] >= idx:
                    continue
                self.seen[eng][f] = idx
                waits.append((self.sems[f], idx))
            else:
                _, slot, val = t
                if self.seen_d[eng][slot] >= val:
                    continue
                self.seen_d[eng][slot] = val
                waits.append((self.dsems[slot], val))
        return waits

    def _commit(self, tok, reads, writes):
        for k in writes:
            self.last_w[k] = tok
            self.readers[k] = []
        for k in reads:
            self.readers.setdefault(k, []).append(tok)

    def op(self, eng, fn, reads=(), writes=()):
        waits = self._waits(eng, reads, writes)
        self.cnt[eng] += 1
        tok = ("e", eng, self.cnt[eng])
        self.q[eng].append((waits, fn, (self.sems[eng], 1)))
        self._commit(tok, reads, writes)

    def dma(self, eng, fn, reads=(), writes=()):
        lo, n_ = (0, 16) if eng == "sp" else (16, NSLOT - 16)
        c = self.dma_q.get(eng, 0)
        self.dma_q[eng] = c + 1
        slot = lo + c % n_
        self.slot_cnt[slot] += 1
        val = 16 * self.slot_cnt[slot]
        extra = [("d", slot, val - 16)] if val > 16 else []
        waits = self._waits(eng, reads, writes, extra)
        tok = ("d", slot, val)
        self.q[eng].append((waits, fn, (self.dsems[slot], 16)))
        self._commit(tok, reads, writes)

    def barrier(self):
        for e in ENG:
            waits = []
            for f in ENG:
                if f != e and self.seen[e][f] < self.cnt[f]:
                    self.seen[e][f] = self.cnt[f]
                    waits.append((self.sems[f], self.cnt[f]))
            for s in range(NSLOT):
                n = self.slot_cnt[s]
                if n > 0 and self.seen_d[e][s] < 16 * n:
                    self.seen_d[e][s] = 16 * n
                    waits.append((self.dsems[s], 16 * n))
            if e != "pe":
                if self.seen[e][e] < self.cnt[e]:
                    self.seen[e][e] = self.cnt[e]
                    waits.append((self.sems[e], self.cnt[e]))
            self.q[e].append((waits, None, None))
        self.last_w = {}
        self.readers = {}

    def flush(self):
        nc = self.nc
        q = self.q

        def mk(name):
            def f(e):
                for waits, fn, inc in q[name]:
                    for sem, val in waits:
                        e.wait_ge(sem, val)
                    if fn is not None:
                        ins = fn(e)
                        ins.then_inc(inc[0], inc[1])
            return f

        with nc.Block() as blk:
            blk.tensor(mk("pe"))
            blk.scalar(mk("act"))
            blk.vector(mk("dve"))
            blk.gpsimd(mk("pool"))
            blk.sync(mk("sp"))
        self.q = {e: [] for e in ENG}


def build_program(stop=None):
    dbg = {}
    nc = bass.Bass("TRN2", target_bir_lowering=False)

    def din(name, shape):
        return nc.dram_tensor(name, list(shape), F32, kind="ExternalInput").ap()

    xT_full = din("xT_full", [D, SEQ])
    xT_q = din("xT_q", [D, NOWN * 128])
    xT_pair = din("xT_pair", [D, NOWN * 256])
    x_own = din("x_own", [NOWN * 128, D])
    pT_own = din("pT_own", [256, NOWN * 128])
    cs_all = din("cs_all", [SEQ, 64])
    csT_own = din("csT_own", [128, 2, NOWN * 128])
    cs_pair = din("cs_pair", [NOWN * 256, 64])
    consts = din("consts", [128, 8, 128])
    w_in = din("w_in", [D, 3776])
    w_uq = din("w_uq", [256, 1536])
    w_ukv = din("w_ukv", [128, 2048])
    w_ukvT = din("w_ukvT", [2048, 128])
    w_out = din("w_out", [D, D])
    w_r = din("w_r", [D, 20])
    b_r = din("b_r", [20])
    w_ei = din("w_ei", [NE, D, 512])
    w_eo = din("w_eo", [NE, 256, D])
    w_pg = din("w_pg", [D, D])
    w_pp = din("w_pp", [256, D])
    g_mixT = din("g_mixT", [128, 8])
    g_q = din("g_q", [256])
    g_kv = din("g_kv", [128])
    g_ffn = din("g_ffn", [D])
    g_ple = din("g_ple", [D])
    g_fin = din("g_fin", [D])
    sinks = din("sinks", [16])
    out_own = nc.dram_tensor("out_own", [NOWN * 128, D], F32, kind="ExternalOutput").ap()

    from contextlib import ExitStack

    with ExitStack() as top:
        uniq = [0]

        def sb(name, shape, dt, st=top):
            uniq[0] += 1
            return st.enter_context(nc.sbuf_tensor("%s_%d" % (name, uniq[0]), list(shape), dt))

        sems = {e: top.enter_context(nc.semaphore("s_" + e)) for e in ENG}
        dsems = [top.enter_context(nc.semaphore("d%d" % i)) for i in range(NSLOT)]
        S = Sched(nc, sems, dsems)
        pb = [top.enter_context(nc.psum_tensor("pb%d" % i, [128, 512], F32)) for i in range(8)]
        PB = lambda i: ("pb", i)

        def mm(out, lhsT, rhs, start, stop, r, w, skip=False):
            S.op("pe", lambda e: e.matmul(out, lhsT, rhs, start=start, stop=stop, skip_group_check=skip), r, w)

        def tr(out, in_, r, w):
            S.op("pe", lambda e: e.transpose(out, in_, ident), list(r) + ["consts"], w)

        def act(out, in_, func, r, w, scale=1.0, bias=None, accum=None):
            kw = {}
            if bias is not None:
                kw["bias"] = bias
            if accum is not None:
                kw["accum_out"] = accum
            S.op("act", lambda e: e.activation(out, in_, func, scale=scale, **kw), r, w)

        def tt(eng, out, a, b, op, r, w):
            S.op(eng, lambda e: e.tensor_tensor(out, a, b, op), r, w)

        def stt(eng, out, in0, scalar, in1, op0, op1, r, w):
            S.op(eng, lambda e: e.scalar_tensor_tensor(out, in0, scalar, in1, op0, op1), r, w)

        def ts(eng, out, in0, s1, s2, op0, op1, r, w):
            S.op(eng, lambda e: e.tensor_scalar(out, in0, s1, s2, op0, op1), r, w)

        def cp(eng, out, in_, r, w):
            if eng == "act":
                S.op("act", lambda e: e.copy(out, in_), r, w)
            else:
                S.op(eng, lambda e: e.tensor_copy(out, in_), r, w)

        def rcp(out, in_, r, w):
            S.op("dve", lambda e: e.reciprocal(out, in_), r, w)

        def rstd(out, in_, n, r, w):
            S.op("pool", lambda e: e.tensor_tensor(out, in_, expo[:, 0:n], ALU.pow), list(r) + ["expo"], w)

        def ms(eng, ap, val, w):
            S.op(eng, lambda e: e.memset(ap, val), (), w)

        def ld(out, in_, w, r=(), eng="sp"):
            S.dma(eng, lambda e: e.dma_start(out=out, in_=in_), r, w)

        def ldc(out, in_, w, r=()):
            S.dma("pool", lambda e: e.dma_start(out=out, in_=in_), r, w)

        def dump(name, ap, shape, dt, rkeys):
            t = nc.dram_tensor("dbg_" + name, list(shape), dt, kind="ExternalOutput").ap()
            ld(t, ap, [("dbg", name)], r=rkeys)

        def finish():
            S.barrier()
            S.flush()
            return nc

        cst = sb("cst", [128, 8, 128], BF16)
        ident = cst[:, 0, :]
        ones = sb("ones", [128, 128], BF16)
        epst = sb("epst", [128, 1], F32)
        gmix = sb("gmix", [128, 8], F32)
        esink = sb("esink", [128, 16], F32)
        o_b = sb("o_b", [128, NOWN, D], BF16)
        wB = sb("wB", [128, 8, 1280], BF16)
        wO = sb("wO", [128, 8, D], BF16)

        ldc(cst[:], consts, ["consts"])
        ms("dve", ones[:], 1.0 / 1024.0, ["ones"])
        ms("dve", epst[:], EPS, ["epst"])
        expo = sb("expo", [128, 512], F32)
        ms("pool", expo[:], -0.5, ["expo"])
        ld(gmix[:], g_mixT, ["gmix"])
        ld(esink[:], sinks.partition_broadcast(128), ["esink"])
        act(esink[:], esink[:], AF.Exp, ["esink"], ["esink"])

        def xn_keys(xn_key):
            return [(xn_key, c) for c in range(8)]

        with ExitStack() as kst:
            KT = sb("KT", [128, 2, SEQ], BF16, kst)
            Vg = sb("Vg", [128, NB, 129], BF16, kst)
            cqT = sb("cqT", [128, 2, NOWN * 128], BF16, kst)
            ms("pool", Vg[:, :, 128:129], 1.0, [("Vg", b) for b in range(NB)])
            wqn = sb("wqn", [128, 2, 8, 128], BF16, kst)
            wqr = sb("wqr", [128, 2, 8, 128], BF16, kst)
            wqt = sb("wqt", [128, 2, 8, 128], BF16, kst)
            wukT = sb("wukT", [128, 8, 128], BF16, kst)
            wuv = sb("wuv", [128, 8, 128], BF16, kst)

            with ExitStack() as ast:
                xb = sb("xb", [128, 3, 8, 512], BF16, ast)
                xsq = sb("xsq", [128, 8, 512], BF16, ast)
                csg = sb("csg", [128, 4, 4, 64], F32, ast)
                wkv = sb("wkv", [128, 8, 192], BF16, ast)
                wcq = sb("wcq", [128, 8, 256], BF16, ast)
                rsA = sb("rsA", [128, 4], F32, ast)
                r4 = sb("r4", [128, 2, 4], F32, ast)
                r2 = sb("r2", [128, 2, 4], F32, ast)
                ssy = sb("ssy", [128, 2, 4], F32, ast)
                sl = sb("sl", [128, 2, 4], F32, ast)
                junk = sb("junk", [128, 256], F32, ast)
                tA = sb("tA", [128, 2, 2, 32], F32, ast)
                tB = sb("tB", [128, 2, 2, 32], F32, ast)
                krt3 = sb("krt3", [128, 3, 128], BF16, ast)
                cqn = sb("cqn", [128, 2, 256], BF16, ast)
                gq_b = sb("gq_b", [128, 256], F32, ast)
                gkv_b = sb("gkv_b", [128, 128], F32, ast)
                ld(gq_b[:], g_q.partition_broadcast(128), ["gq_b"])
                ld(gkv_b[:], g_kv.partition_broadcast(128), ["gkv_b"])

                w_in_v = w_in.rearrange("(c p) f -> p c f", p=128)
                ldc(wkv[:], w_in_v[:, :, 1536:1728], ["wkv"])
                ldc(wcq[:], w_in_v[:, :, 1280:1536], ["wcq"])
                ms("dve", krt3[:], 0.0, [("krt", 0), ("krt", 1), ("krt", 2)])

                xTf = xT_full.rearrange("(c p) t -> p c t", p=128)
                csa = cs_all.rearrange("(b p) f -> p b f", p=128)
                xTq = xT_q.rearrange("(c p) t -> p c t", p=128)
                groups = [("kv", g) for g in range(NB // 4)] + [("cq", g) for g in range(NOWN // 4)]
                NG = len(groups)
                YB = ((1, 2), (5, 6))

                def g_load(gi):
                    kind, g = groups[gi]
                    k3 = gi % 3
                    if kind == "kv":
                        ldc(xb[:, k3], xTf[:, :, g * 512:(g + 1) * 512], [("xb", k3)])
                        ld(csg[:, gi % 4], csa[:, 4 * g:4 * g + 4, :], [("csg", gi % 4)])
                    else:
                        ldc(xb[:, k3], xTq[:, :, g * 512:(g + 1) * 512], [("xb", k3)])

                def g_sq(gi):
                    act(xsq[:], xb[:, gi % 3], AF.Square, [("xb", gi % 3)], ["xsq"])

                def g_stats(gi):
                    k = gi % 2
                    for b_ in range(4):
                        for c in range(8):
                            mm(pb[0][:, b_:b_ + 1], xsq[:, c, b_ * 128:(b_ + 1) * 128], ones[:, 0:1], b_ == 0 and c == 0, c == 7,
                               ["ones", "xsq"], [PB(0)], skip=True)
                    act(rsA[:, 0:4], pb[0][:, 0:4], AF.Identity, [PB(0), "epst"], ["rsA"], bias=epst[:, 0:1])
                    rstd(r4[:, k, :], rsA[:, 0:4], 4, ["rsA"], [("r4", k)])
                    tt("pool", r2[:, k, :], r4[:, k, :], r4[:, k, :], ALU.mult, [("r4", k)], [("r2", k)])

                def g_mm(gi):
                    kind, g = groups[gi]
                    k = gi % 2
                    k3 = gi % 3
                    w_, wkey, W, Wn = (wkv, "wkv", 192, 128) if kind == "kv" else (wcq, "wcq", 256, 256)
                    ms("dve", ssy[:, k, :], 0.0, [("ssy", k)])
                    for b_ in range(4):
                        bank = YB[k][b_ // 2]
                        off = (b_ % 2) * W
                        for c in range(8):
                            mm(pb[bank][:, off:off + W], xb[:, k3, c, b_ * 128:(b_ + 1) * 128], w_[:, c, :],
                               b_ % 2 == 0 and c == 0, c == 7, [("xb", k3), wkey], [PB(bank)], skip=True)
                    for b_ in range(4):
                        bank = YB[k][b_ // 2]
                        off = (b_ % 2) * W
                        act(junk[:, 0:Wn], pb[bank][:, off:off + Wn], AF.Square, [PB(bank)], ["junk", ("ssy", k)],
                            accum=ssy[:, k, b_:b_ + 1], scale=float(Wn ** -0.5))

                def g_sc_a(gi):
                    k = gi % 2
                    tt("dve", sl[:, k, :], ssy[:, k, :], r2[:, k, :], ALU.mult, [("ssy", k), ("r2", k)], [("sl", k)])
                    ts("dve", sl[:, k, :], sl[:, k, :], EPS, None, ALU.add, ALU.bypass, [("sl", k)], [("sl", k)])
                    rstd(sl[:, k, :], sl[:, k, :], 4, [("sl", k)], [("sl", k)])

                def g_sc_b(gi):
                    k = gi % 2
                    tt("dve", sl[:, k, :], sl[:, k, :], r4[:, k, :], ALU.mult, [("sl", k), ("r4", k)], [("sl", k)])

                def g_ev(gi, b_):
                    kind, g = groups[gi]
                    k = gi % 2
                    t = 4 * gi + b_
                    kk = t % 2
                    k3 = t % 3
                    blk = 4 * g + b_
                    bank = YB[k][b_ // 2]
                    if kind == "kv":
                        off = (b_ % 2) * 192
                        stt("dve", Vg[:, blk, 0:128], pb[bank][:, off:off + 128], sl[:, k, b_:b_ + 1], gkv_b[:],
                            ALU.mult, ALU.mult, [PB(bank), ("sl", k), "gkv_b"], [("Vg", blk)])
                        kr = pb[bank][:, off + 128:off + 192].rearrange("p (t f) -> p t f", t=2)
                        cosb = csg[:, gi % 4, b_, 0:32].unsqueeze(1).to_broadcast([128, 2, 32])
                        sinb = csg[:, gi % 4, b_, 32:64].unsqueeze(1).to_broadcast([128, 2, 32])
                        stt("dve", tA[:, kk], kr, r4[:, k, b_:b_ + 1], cosb, ALU.mult, ALU.mult,
                            [PB(bank), ("csg", gi % 4), ("r4", k)], [("tA", kk)])
                        stt("dve", tB[:, kk], kr, r4[:, k, b_:b_ + 1], sinb, ALU.mult, ALU.mult,
                            [PB(bank), ("csg", gi % 4), ("r4", k)], [("tB", kk)])
                        tt("dve", krt3[:, k3, 0:32], tA[:, kk, 0, :], tB[:, kk, 1, :], ALU.subtract,
                           [("tA", kk), ("tB", kk)], [("krt", k3)])
                        tt("dve", krt3[:, k3, 32:64], tA[:, kk, 1, :], tB[:, kk, 0, :], ALU.add,
                           [("tA", kk), ("tB", kk)], [("krt", k3)])
                    else:
                        off = (b_ % 2) * 256
                        stt("dve", cqn[:, kk, :], pb[bank][:, off:off + 256], sl[:, k, b_:b_ + 1], gq_b[:], ALU.mult, ALU.mult,
                            [PB(bank), ("sl", k), "gq_b"], [("cqn", kk)])

                def g_tr(gi, b_):
                    kind, g = groups[gi]
                    t = 4 * gi + b_
                    kk = t % 2
                    k3 = t % 3
                    blk = 4 * g + b_
                    tbank = 3 + kk
                    tp = pb[tbank][:].bitcast(BF16)
                    if kind == "kv":
                        tr(tp[:, 0:128], Vg[:, blk, 0:128], [("Vg", blk)], [PB(tbank)])
                        tr(tp[:, 128:256], krt3[:, k3, :], [("krt", k3)], [PB(tbank)])
                        cp("dve" if b_ % 2 == 0 else "act", KT[:, :, blk * 128:(blk + 1) * 128],
                           tp[:, 0:256].rearrange("p (t f) -> p t f", t=2), [PB(tbank)], [("KT", blk)])
                    else:
                        tr(tp[:, 0:128], cqn[:, kk, 0:128], [("cqn", kk)], [PB(tbank)])
                        tr(tp[:, 128:256], cqn[:, kk, 128:256], [("cqn", kk)], [PB(tbank)])
                        cp("dve" if b_ % 2 == 0 else "act", cqT[:, :, blk * 128:(blk + 1) * 128],
                           tp[:, 0:256].rearrange("p (t f) -> p t f", t=2), [PB(tbank)], [("cqT", blk)])

                g_load(0)
                g_load(1)
                g_load(2)
                wq_v = w_uq.rearrange("(c p) (h f) -> p c h f", p=128, f=192)
                for c in range(2):
                    ldc(wqn[:, c], wq_v[:, c, :, 0:128], ["wqn"])
                ms("pool", wqr[:], 0.0, ["wqr"])
                ms("pool", wqt[:], 0.0, ["wqt"])
                for c in range(2):
                    ldc(wqr[:, c, :, 0:64], wq_v[:, c, :, 128:192], ["wqr"])
                ldc(wukT[:], w_ukvT.rearrange("(h t n) r -> n h t r", t=2, n=128)[:, :, 0, :], ["wukT"])
                ldc(wuv[:], w_ukv.rearrange("r (h t v) -> r h t v", t=2, v=128)[:, :, 1, :], ["wuv"])
                for c in range(8):
                    ts("dve", wkv[:, c, :], wkv[:, c, :], gmix[:, c:c + 1], None, ALU.mult, ALU.bypass, ["wkv", "gmix"], ["wkv"])
                    ts("dve", wcq[:, c, :], wcq[:, c, :], gmix[:, c:c + 1], None, ALU.mult, ALU.bypass, ["wcq", "gmix"], ["wcq"])
                g_sq(0)
                g_stats(0)
                g_mm(0)
                g_sc_a(0)
                g_sq(1)
                g_sc_b(0)
                g_stats(1)
                for gi in range(NG):
                    if gi == 4:
                        ts("pool", wqt[:, :, :, 0:32], wqr[:, :, :, 32:64], -1.0, None, ALU.mult, ALU.bypass, ["wqr"], ["wqt"])
                        cp("pool", wqt[:, :, :, 32:64], wqr[:, :, :, 0:32], ["wqr"], ["wqt"])
                    if gi + 3 < NG:
                        g_load(gi + 3)
                    if gi + 2 < NG:
                        g_sq(gi + 2)
                    if gi + 1 < NG:
                        g_mm(gi + 1)
                    for b_ in range(5):
                        if b_ < 4:
                            g_ev(gi, b_)
                        if b_ >= 1:
                            g_tr(gi, b_ - 1)
                    if gi + 1 < NG:
                        g_sc_a(gi + 1)
                    if gi + 2 < NG:
                        g_stats(gi + 2)
                    if gi + 1 < NG:
                        g_sc_b(gi + 1)
                if stop == "A":
                    dump("KT", KT[:], [128, 2, SEQ], BF16, [("KT", b) for b in range(NB)])
                    dump("Vg", Vg[:], [128, NB, 129], BF16, [("Vg", b) for b in range(NB)])
                    dump("cqT", cqT[:], [128, 2, NOWN * 128], BF16, [("cqT", b) for b in range(NOWN)])
                    return finish()
                S.barrier()
                S.flush()

            with ExitStack() as mst:
                qabs = sb("qabs", [128, 2, 8, 512], BF16, mst)
                qrope = sb("qrope", [128, 2, 8, 512], BF16, mst)
                qn_sb = sb("qn_sb", [128, 2, 512], BF16, mst)
                csT = sb("csT", [128, 2, 2, 512], F32, mst)
                t1 = sb("t1", [128, 2, 512], F32, mst)
                t2 = sb("t2", [128, 2, 512], F32, mst)
                pT = sb("pT", [128, 4, 512], BF16, mst)
                rc = sb("rc", [128, 8], F32, mst)
                olat = sb("olat", [128, 2, D], BF16, mst)
                olT = sb("olT", [128, 2, D], BF16, mst)

                ldc(wB[:], w_in_v[:, :, 0:1280], ["wB"])
                ldc(wO[:], w_out.rearrange("(c p) f -> p c f", p=128), ["wO"])

                LOOK = 2

                def qprep(ig, h, part):
                    qk = ig % 2
                    cols = slice(ig * 512, (ig + 1) * 512)
                    hk2 = h % 2
                    cq_keys = [("cqT", 4 * ig + b_) for b_ in range(4)]
                    if part == 0:
                        if h == 0:
                            ld(csT[:, qk], csT_own[:, :, cols], [("csT", qk)])
                        for c in range(2):
                            mm(pb[6][:], wqn[:, c, h, :], cqT[:, c, cols], c == 0, c == 1, ["wqn"] + cq_keys, [PB(6)])
                        cp("dve", qn_sb[:, hk2, :], pb[6][:], [PB(6)], [("qn_sb", hk2)])
                    elif part == 1:
                        for c in range(2):
                            mm(pb[7][:], wqr[:, c, h, :], cqT[:, c, cols], c == 0, c == 1, ["wqr"] + cq_keys, [PB(7)])
                        tt("dve", t1[:, hk2, :], pb[7][:], csT[:, qk, 0, :], ALU.mult, [PB(7), ("csT", qk)], [("t1", hk2)])
                    elif part == 2:
                        mm(pb[6][:], wukT[:, h, :], qn_sb[:, hk2, :], True, True, ["wukT", ("qn_sb", hk2)], [PB(6)])
                        cp("dve", qabs[:, qk, h, :], pb[6][:], [PB(6)], [("qabs", qk, h)])
                    else:
                        for c in range(2):
                            mm(pb[7][:], wqt[:, c, h, :], cqT[:, c, cols], c == 0, c == 1, ["wqt"] + cq_keys, [PB(7)])
                        tt("dve", t2[:, hk2, :], pb[7][:], csT[:, qk, 1, :], ALU.mult, [PB(7), ("csT", qk)], [("t2", hk2)])
                        tt("pool", qrope[:, qk, h, :], t1[:, hk2, :], t2[:, hk2, :], ALU.add,
                           [("t1", hk2), ("t2", hk2)], [("qrope", qk, h)])

                def evac_a(i):
                    ok = i % 2
                    for bk in range(3):
                        nh = 3 if bk < 2 else 2
                        ov = pb[3 + bk][:, 0:nh * 129].rearrange("p (h f) -> p h f", f=129)
                        rcp(rc[:, 3 * bk:3 * bk + nh], ov[:, :, 128], [PB(3 + bk)], ["rc"])
                        tt("dve", olat[:, ok, 384 * bk:384 * bk + nh * 128].rearrange("p (h f) -> p h f", f=128),
                           ov[:, :, 0:128], rc[:, 3 * bk:3 * bk + nh].unsqueeze(2).to_broadcast([128, nh, 128]),
                           ALU.mult, [PB(3 + bk), "rc"], [("olat", ok)])

                def evac_b(i):
                    ok = i % 2
                    tp = pb[6][:].bitcast(BF16)
                    for h in range(8):
                        tr(tp[:, h * 128:(h + 1) * 128], olat[:, ok, h * 128:(h + 1) * 128], [("olat", ok)], [PB(6)])
                    cp("dve", olT[:, ok, :], tp[:, :], [PB(6)], [("olT", ok)])

                def evac_c(i, part):
                    ok = i % 2
                    for h in range(4 * part, 4 * part + 4):
                        mm(pb[7][:, (h % 4) * 128:(h % 4 + 1) * 128], olT[:, ok, h * 128:(h + 1) * 128], wuv[:, h, :],
                           True, True, [("olT", ok), "wuv"], [PB(7)])
                    cp("dve", o_b[:, i, 512 * part:512 * (part + 1)], pb[7][:], [PB(7)], [("o_b", i)])

                steps = []
                for i in range(NOWN):
                    for kb in range(4 * i + 4):
                        for hg in range(2):
                            steps.append((i, kb, hg))
                NS_ = len(steps)
                deferred = {}

                def defer(at, fn):
                    deferred.setdefault(min(at, NS_ - 1), []).append(fn)

                for h in range(8):
                    for part in range(4):
                        qprep(0, h, part)
                first_of_ig = {}
                for n, (i, kb, hg) in enumerate(steps):
                    if kb == 0 and hg == 0 and i % 4 == 0:
                        first_of_ig[i // 4] = n
                for ig in range(1, 4):
                    n0 = first_of_ig[ig - 1] + 4
                    for h in range(8):
                        for part in range(4):
                            defer(n0 + 2 * (4 * h + part), (lambda ig=ig, h=h, part=part: qprep(ig, h, part)))

                def s_stage(n):
                    i, kb, hg = steps[n]
                    qk = (i // 4) % 2
                    qc = slice((i % 4) * 128, (i % 4 + 1) * 128)
                    kcol = slice(kb * 128, (kb + 1) * 128)
                    sbk = n % 3
                    pk = n % 4
                    qkeys = [("qabs", qk, h) for h in range(4 * hg, 4 * hg + 4)]
                    rkeys = [("qrope", qk, h) for h in range(4 * hg, 4 * hg + 4)]
                    mm(pb[sbk][:].rearrange("p (h q) -> p h q", h=4), KT[:, 0, kcol], qabs[:, qk, 4 * hg:4 * hg + 4, qc], True, False,
                       [("KT", kb)] + qkeys, [PB(sbk)])
                    mm(pb[sbk][:].rearrange("p (h q) -> p h q", h=4), KT[:, 1, kcol], qrope[:, qk, 4 * hg:4 * hg + 4, qc], False, True,
                       [("KT", kb)] + rkeys, [PB(sbk)])
                    act(pT[:, pk, :], pb[sbk][:], AF.Exp, [PB(sbk)], [("pT", pk)], scale=float(MLA_SCALE))
                    if kb >= 4 * i:
                        m = kb - 4 * i
                        pv = pT[:, pk, :].rearrange("p (h q) -> p h q", h=4)
                        tt("dve", pv, pv, cst[:, 1 + m, :].unsqueeze(1).to_broadcast([128, 4, 128]), ALU.mult,
                           [("pT", pk), "consts"], [("pT", pk)])

                def p_stage(n):
                    i, kb, hg = steps[n]
                    nkb = 4 * i + 4
                    pk = n % 4
                    for hh in range(4):
                        h = 4 * hg + hh
                        ob = 3 + h // 3
                        oc = (h % 3) * 129
                        mm(pb[ob][:, oc:oc + 129], pT[:, pk, hh * 128:(hh + 1) * 128], Vg[:, kb, :],
                           kb == 0 and h % 3 == 0, kb == nkb - 1, [("pT", pk), ("Vg", kb)], [PB(ob)], skip=True)
                    if kb == nkb - 1 and hg == 1:
                        evac_a(i)
                        defer(n + 3, lambda i=i: evac_b(i))
                        defer(n + 5, lambda i=i: evac_c(i, 0))
                        defer(n + 7, lambda i=i: evac_c(i, 1))

                for n in range(NS_ + LOOK):
                    if n < NS_:
                        s_stage(n)
                    m_ = n - LOOK
                    if m_ >= 0:
                        p_stage(m_)
                        for fn in deferred.pop(m_, []):
                            fn()
                for k_ in sorted(deferred):
                    for fn in deferred[k_]:
                        fn()
                for c in range(8):
                    ts("dve", wB[:, c, :], wB[:, c, :], gmix[:, c:c + 1], None, ALU.mult, ALU.bypass, ["wB", "gmix"], ["wB"])
                if stop == "M":
                    dump("o_b", o_b[:], [128, NOWN, D], BF16, [("o_b", b) for b in range(NOWN)])
                    return finish()
                S.barrier()
                S.flush()

        wG = sb("wG", [128, 8, 2048], BF16)
        for half in range(2):
            with ExitStack() as hst:
                hres = sb("hres", [128, 8, D], F32, hst)
                with ExitStack() as gst:
                    xpb = sb("xpb", [128, 2, 8, 256], BF16, gst)
                    rr = sb("rr", [128, 2, 4], F32, gst)
                    csp = sb("csp", [128, 2, 2, 64], F32, gst)
                    xsq = sb("xsq2", [128, 8, 256], BF16, gst)
                    rs = sb("rs2", [128, 4], F32, gst)
                    qA = sb("qA", [128, D], F32, gst)
                    qB = sb("qB", [128, D], F32, gst)
                    qar = sb("qar", [128, D], BF16, gst)
                    kA = sb("kA", [128, 2, 128], F32, gst)
                    kB = sb("kB", [128, 2, 128], F32, gst)
                    kpad = sb("kpad", [128, 2, 4, 128], BF16, gst)
                    vaug = sb("vaug", [128, 2, 2, 65], BF16, gst)
                    qaT = sb("qaT", [128, 8, 128], BF16, gst)
                    kT = sb("kT", [128, 8, 128], BF16, gst)
                    pS = sb("pS", [128, 8, 512], BF16, gst)
                    den = sb("den", [128, 16], F32, gst)
                    oa = sb("oa", [128, 16, 64], F32, gst)
                    m1 = sb("m1", [128, D], BF16, gst)
                    m2 = sb("m2", [128, D], BF16, gst)
                    mg = sb("mg", [128, D], BF16, gst)
                    mgT = sb("mgT", [128, 8, 128], BF16, gst)

                    w_in_v = w_in.rearrange("(c p) f -> p c f", p=128)
                    ms("pool", kpad[:], 0.0, [("kpad", t_, p_) for t_ in range(2) for p_ in range(2)])
                    ms("pool", vaug[:, :, :, 64:65], 1.0, ["vaug"])
                    xTp = xT_pair.rearrange("(c p) t -> p c t", p=128)
                    csp_v = cs_pair.rearrange("(b p) f -> p b f", p=128)
                    th2 = sb("th2", [128, 2, 2048], BF16, gst)

                    def pg_load(ii):
                        i = half * 8 + ii
                        k = ii % 2
                        ldc(xpb[:, k], xTp[:, :, i * 256:(i + 1) * 256], [("xpb", k)])
                        ld(csp[:, k], csp_v[:, 2 * i:2 * i + 2, :], [("csp", k)])
                        ld(hres[:, ii, :], x_own[i * 128:(i + 1) * 128, :], [("hres", ii)])

                    def pg_sq(ii):
                        k = ii % 2
                        act(xsq[:], xpb[:, k], AF.Square, [("xpb", k)], ["xsq"])

                    def pg_x1a(ii):
                        k = ii % 2
                        for t in range(2):
                            for c in range(8):
                                mm(pb[1][:, t:t + 1], xsq[:, c, t * 128:(t + 1) * 128], ones[:, 0:1], t == 0 and c == 0, c == 7,
                                   ["ones", "xsq"], [PB(1)], skip=True)
                        act(rs[:, 0:2], pb[1][:, 0:2], AF.Identity, [PB(1), "epst"], ["rs"], bias=epst[:, 0:1])
                        rstd(rr[:, k, 0:2], rs[:, 0:2], 2, ["rs"], [("rr", k)])
                        ts("pool", rr[:, k, 2:3], rr[:, k, 1:2], 0.5, None, ALU.mult, ALU.bypass, [("rr", k)], [("rr", k)])
                        if ii + 1 < 8:
                            pg_load(ii + 1)

                    def pg_gate(ii, q4, parts=(0, 1), bank=None):
                        k = ii % 2
                        gb = 6 + (q4 % 2) if bank is None else bank
                        for part in parts:
                            for c in range(4 * part, 4 * part + 4):
                                mm(pb[gb][:], xpb[:, k, c, 128:256], wG[:, c, q4 * 512:(q4 + 1) * 512], c == 0, c == 7,
                                   [("xpb", k), ("wG", c)], [PB(gb)])
                            if part == 1:
                                act(th2[:, k, q4 * 512:(q4 + 1) * 512], pb[gb][:], AF.Tanh, [PB(gb), ("rr", k)], [("th", k, q4)],
                                    scale=rr[:, k, 2:3])

                    def pg_x2(ii):
                        k = ii % 2
                        for t in range(2):
                            for c in range(8):
                                mm(pb[5][:, t * 256:(t + 1) * 256], xpb[:, k, c, t * 128:(t + 1) * 128], wB[:, c, 1024:1280],
                                   c == 0, c == 7, [("xpb", k), "wB"], [PB(5)])
                        for hf in range(2):
                            for c in range(8):
                                mm(pb[6 + hf][:], xpb[:, k, c, 128:256], wB[:, c, hf * 512:(hf + 1) * 512], c == 0, c == 7,
                                   [("xpb", k), "wB"], [PB(6 + hf)])
                        m3 = lambda a: a.rearrange("p h t f -> p (h t) f")
                        for t in range(2):
                            kv = pb[5][:, t * 256:t * 256 + 128].rearrange("p (h t f) -> p h t f", h=2, t=2)
                            kAv = kA[:, t, :].rearrange("p (h t f) -> p h t f", h=2, t=2)
                            kBv = kB[:, t, :].rearrange("p (h t f) -> p h t f", h=2, t=2)
                            cos3 = csp[:, k, t, 0:32].unsqueeze(1).to_broadcast([128, 4, 32])
                            sin3 = csp[:, k, t, 32:64].unsqueeze(1).to_broadcast([128, 4, 32])
                            stt("dve", m3(kAv), m3(kv), rr[:, k, t:t + 1], cos3, ALU.mult, ALU.mult,
                                [PB(5), ("csp", k), ("rr", k)], [("kA", t)])
                            stt("dve", m3(kBv), m3(kv), rr[:, k, t:t + 1], sin3, ALU.mult, ALU.mult,
                                [PB(5), ("csp", k), ("rr", k)], [("kB", t)])
                            ts("dve", vaug[:, t, :, 0:64], pb[5][:, t * 256 + 128:(t + 1) * 256].rearrange("p (h f) -> p h f", h=2),
                               rr[:, k, t:t + 1], None, ALU.mult, ALU.bypass, [PB(5), ("rr", k)], ["vaug"])
                            kp5 = kpad[:, t].rearrange("p (hk par) d -> p hk par d", par=2)
                            for par in range(2):
                                kpv = kp5[:, :, par, par * 64:(par + 1) * 64].rearrange("p h (t f) -> p h t f", t=2)
                                tt("dve", kpv[:, :, 0, :], kAv[:, :, 0, :], kBv[:, :, 1, :], ALU.subtract,
                                   [("kA", t), ("kB", t)], [("kpad", t, par)])
                                tt("dve", kpv[:, :, 1, :], kAv[:, :, 1, :], kBv[:, :, 0, :], ALU.add,
                                   [("kA", t), ("kB", t)], [("kpad", t, par)])
                        cos3 = csp[:, k, 1, 0:32].unsqueeze(1).to_broadcast([128, 16, 32])
                        sin3 = csp[:, k, 1, 32:64].unsqueeze(1).to_broadcast([128, 16, 32])
                        for hf in range(2):
                            qv = pb[6 + hf][:].rearrange("p (h t f) -> p h t f", h=8, t=2)
                            qAv = qA[:, hf * 512:(hf + 1) * 512].rearrange("p (h t f) -> p h t f", h=8, t=2)
                            qBv = qB[:, hf * 512:(hf + 1) * 512].rearrange("p (h t f) -> p h t f", h=8, t=2)
                            stt("dve", m3(qAv), m3(qv), rr[:, k, 1:2], cos3, ALU.mult, ALU.mult,
                                [PB(6 + hf), ("csp", k), ("rr", k)], [("qA", hf)])
                            stt("dve", m3(qBv), m3(qv), rr[:, k, 1:2], sin3, ALU.mult, ALU.mult,
                                [PB(6 + hf), ("csp", k), ("rr", k)], [("qB", hf)])
                        qA4 = qA[:].rearrange("p (h t f) -> p h t f", h=16, t=2)
                        qB4 = qB[:].rearrange("p (h t f) -> p h t f", h=16, t=2)
                        qar4 = qar[:].rearrange("p (h t f) -> p h t f", h=16, t=2)
                        tt("dve", qar4[:, :, 0, :], qA4[:, :, 0, :], qB4[:, :, 1, :], ALU.subtract,
                           [("qA", 0), ("qA", 1), ("qB", 0), ("qB", 1)], ["qar"])
                        tt("dve", qar4[:, :, 1, :], qA4[:, :, 1, :], qB4[:, :, 0, :], ALU.add,
                           [("qA", 0), ("qA", 1), ("qB", 0), ("qB", 1)], ["qar"])

                    def pg_x3(ii, gates=False):
                        tp2 = pb[5][:].bitcast(BF16)
                        for t in range(2):
                            for v in range(4):
                                tr(tp2[:, (t * 4 + v) * 128:(t * 4 + v + 1) * 128], kpad[:, t, v, :], [("kpad", t, v % 2)], [PB(5)])
                        cp("dve", kT[:].rearrange("p c f -> p (c f)"), tp2[:, :], [PB(5)], ["kT"])
                        if gates:
                            pg_gate(ii, 2)
                        tp = pb[0][:].bitcast(BF16)
                        for c in range(8):
                            tr(tp[:, c * 128:(c + 1) * 128], qar[:, c * 128:(c + 1) * 128], ["qar"], [PB(0)])
                        cp("act", qaT[:].rearrange("p c f -> p (c f)"), tp[:, :], [PB(0)], ["qaT"])
                        if gates:
                            pg_gate(ii, 3)

                    def pg_y1(ii, nxt):
                        i = half * 8 + ii
                        sidx = 0
                        for hk in range(2):
                            for par in range(2):
                                for t in range(2):
                                    sbk = sidx % 2
                                    mm(pb[sbk][:].rearrange("p (h q) -> p h q", h=4), kT[:, t * 4 + hk * 2 + par, :],
                                       qaT[:, 4 * hk:4 * hk + 4, :], True, True, ["kT", "qaT"], [PB(sbk)])
                                    act(pS[:, sidx, :], pb[sbk][:], AF.Exp, [PB(sbk)], [("pS", sidx)], scale=float(SWA_SCALE))
                                    mi = 7 if t == 1 else (5 if i == 0 else 6)
                                    pv = pS[:, sidx, :].rearrange("p (h q) -> p h q", h=4)
                                    tt("dve", pv, pv,
                                       cst[:, mi, :].unsqueeze(1).to_broadcast([128, 4, 128]), ALU.mult,
                                       [("pS", sidx), "consts"], [("pS", sidx)])
                                    sidx += 1
                                    if nxt and sidx in (2, 4):
                                        pg_gate(ii + 1, 0, parts=(sidx // 2 - 1,))

                    def pg_y2(ii):
                        i = half * 8 + ii
                        k = ii % 2
                        started = set()
                        sidx = 0
                        for hk in range(2):
                            for par in range(2):
                                for t in range(2):
                                    for ci in range(4):
                                        head = 2 * (4 * hk + ci) + par
                                        ob = 2 + head // 7
                                        oc = (head % 7) * 65
                                        first = ob not in started
                                        started.add(ob)
                                        mm(pb[ob][:, oc:oc + 65], pS[:, sidx, ci * 128:(ci + 1) * 128], vaug[:, t, hk, :],
                                           first, t == 1, [("pS", sidx), "vaug"], [PB(ob)], skip=True)
                                    sidx += 1
                        for bk in range(3):
                            nh = 7 if bk < 2 else 2
                            ov = pb[2 + bk][:, 0:nh * 65].rearrange("p (h f) -> p h f", f=65)
                            tt("dve", den[:, 7 * bk:7 * bk + nh], ov[:, :, 64], esink[:, 7 * bk:7 * bk + nh], ALU.add,
                               [PB(2 + bk), "esink"], [("den", bk)])
                            rcp(den[:, 7 * bk:7 * bk + nh], den[:, 7 * bk:7 * bk + nh], [("den", bk)], [("den", bk)])
                            tt("dve", oa[:, 7 * bk:7 * bk + nh, :], ov[:, :, 0:64],
                               den[:, 7 * bk:7 * bk + nh].unsqueeze(2).to_broadcast([128, nh, 64]), ALU.mult,
                               [PB(2 + bk), ("den", bk)], [("oa", bk)])
                        oaf = oa[:].rearrange("p h f -> p (h f)")
                        stt("dve", m1[:], th2[:, k, 0:1024], 1.0, oaf, ALU.add, ALU.mult,
                            [("th", k, 0), ("th", k, 1), ("oa", 0), ("oa", 1), ("oa", 2)], ["m1"])
                        stt("dve", m2[:], th2[:, k, 1024:2048], 1.0, o_b[:, i, :], ALU.add, ALU.mult,
                            [("th", k, 2), ("th", k, 3), ("o_b", i)], ["m2"])
                        tt("dve", mg[:], m1[:], m2[:], ALU.add, ["m1", "m2"], ["mg"])

                    def pg_y3(ii, nxt=False):
                        if nxt:
                            pg_gate(ii + 1, 1, parts=(0,), bank=0)
                        tp = pb[1][:].bitcast(BF16)
                        for c in range(8):
                            tr(tp[:, c * 128:(c + 1) * 128], mg[:, c * 128:(c + 1) * 128], ["mg"], [PB(1)])
                        cp("act", mgT[:].rearrange("p c f -> p (c f)"), tp[:, :], [PB(1)], ["mgT"])
                        if nxt:
                            pg_gate(ii + 1, 1, parts=(1,), bank=0)

                    def pg_y4(ii):
                        for hf in range(2):
                            for c in range(8):
                                mm(pb[2 + hf][:], mgT[:, c, :], wO[:, c, hf * 512:(hf + 1) * 512], c == 0, c == 7,
                                   ["mgT", "wO"], [PB(2 + hf)])
                            stt("dve", hres[:, ii, hf * 512:(hf + 1) * 512], pb[2 + hf][:], 0.5,
                                hres[:, ii, hf * 512:(hf + 1) * 512], ALU.mult, ALU.add,
                                [PB(2 + hf), ("hres", ii)], [("hres", ii)])

                    pg_load(0)
                    if half == 0:
                        for c in range(8):
                            ldc(wG[:, c, :], w_in_v[:, c, 1728:3776], [("wG", c)])
                    pg_sq(0)
                    pg_x1a(0)
                    pg_x2(0)
                    pg_x3(0)
                    if half == 0:
                        for c in range(8):
                            ts("dve", wG[:, c, :], wG[:, c, :], gmix[:, c:c + 1], None, ALU.mult, ALU.bypass,
                               [("wG", c), "gmix"], [("wG", c)])
                    for q4 in range(4):
                        pg_gate(0, q4)
                    pg_sq(1)
                    for ii in range(8):
                        nxt = ii + 1 < 8
                        if nxt:
                            pg_x1a(ii + 1)
                        pg_y1(ii, nxt)
                        pg_y2(ii)
                        if ii + 2 < 8:
                            pg_sq(ii + 2)
                        if nxt:
                            pg_x2(ii + 1)
                        pg_y3(ii, nxt)
                        pg_y4(ii)
                        if nxt:
                            pg_x3(ii + 1, gates=True)
                    if stop == "G" and half == 0:
                        dump("hres", hres[:], [128, 8, D], F32, [("hres", b) for b in range(8)])
                        return finish()
                    S.barrier()
                    S.flush()

                with ExitStack() as est:
                    hnT = sb("hnT", [128, 8, 8 * 128], BF16, est)
                    hn = sb("hn", [128, 2, D], BF16, est)
                    junk2 = sb("junk2", [128, D], F32, est)
                    ssh = sb("ssh", [128, 8], F32, est)
                    wr = sb("wr", [128, 8, 20], BF16, est)
                    lg = sb("lg", [128, 8, 20], F32, est)
                    gmx = sb("gmx", [128, 8], F32, est)
                    oh = sb("oh", [128, 8, 4], F32, est)
                    ge = sb("ge", [128, 8, 4], F32, est)
                    gs = sb("gs", [128, 8], F32, est)
                    esel4 = sb("esel4", [128, 8, 4, 4], F32, est)
                    esel = sb("esel", [128, 8, 4], F32, est)
                    e1 = sb("e1", [128, 8], F32, est)
                    em = sb("em", [128, 8, 4], F32, est)
                    e2 = sb("e2", [128, 8], F32, est)
                    sel = sb("sel", [128, 8, 4], F32, est)
                    ew = sb("ew", [128, 8, 4], F32, est)
                    es = sb("es", [128, 8], F32, est)
                    cmb = sb("cmb", [128, 8, 16], F32, est)
                    wei = sb("wei", [128, 2, 8, 512], BF16, est)
                    weo = sb("weo", [128, 2, 2, D], BF16, est)
                    sg = sb("sg", [128, 2, 256], F32, est)
                    ac = sb("ac", [128, 2, 256], BF16, est)
                    acT = sb("acT", [128, 2, 256], BF16, est)
                    gffn_b = sb("gffn_b", [128, D], F32, est)
                    br_b = sb("br_b", [128, 20], F32, est)
                    ld(gffn_b[:], g_ffn.partition_broadcast(128), ["gffn_b"])
                    ld(br_b[:], b_r.partition_broadcast(128), ["br_b"])

                    ldc(wr[:], w_r.rearrange("(c p) f -> p c f", p=128), ["wr"])
                    wei_v = w_ei.rearrange("e (c p) f -> e p c f", p=128)
                    weo_v = w_eo.rearrange("e (c p) f -> e p c f", p=128)
                    for ex_ in range(2):
                        ldc(wei[:, ex_], wei_v[ex_], [("wei", ex_)])
                        ldc(weo[:, ex_], weo_v[ex_], [("weo", ex_)])
                    ms("dve", ssh[:], EPS, [("ssh", ii_) for ii_ in range(8)])

                    def pre_na(ii):
                        act(junk2[:], hres[:, ii, :], AF.Square, [("hres", ii)], ["junk2", ("ssh", ii)], accum=ssh[:, ii:ii + 1],
                            scale=float(D ** -0.5))
                        rstd(ssh[:, ii:ii + 1], ssh[:, ii:ii + 1], 1, [("ssh", ii)], [("ssh", ii)])

                    def pre_nb(ii):
                        kk = ii % 2
                        stt("dve", hn[:, kk, :], hres[:, ii, :], ssh[:, ii:ii + 1], gffn_b[:], ALU.mult, ALU.mult,
                            [("hres", ii), ("ssh", ii), "gffn_b"], [("hn", kk)])

                    def pre_t(ii):
                        kk = ii % 2
                        tbank = 4 + kk
                        tp = pb[tbank][:].bitcast(BF16)
                        for c in range(8):
                            tr(tp[:, c * 128:(c + 1) * 128], hn[:, kk, c * 128:(c + 1) * 128], [("hn", kk)], [PB(tbank)])
                        cp("act", hnT[:, :, ii * 128:(ii + 1) * 128], tp[:, :].rearrange("p (c f) -> p c f", c=8),
                           [PB(tbank)], [("hnT", ii)])

                    def pre_r(ii):
                        for c in range(8):
                            mm(pb[6][:, ii * 20:(ii + 1) * 20], hnT[:, c, ii * 128:(ii + 1) * 128], wr[:, c, :], c == 0, c == 7,
                               [("hnT", ii), "wr"], [PB(6)])

                    pre_na(0)
                    pre_na(1)
                    for t in range(8 + 2):
                        if t + 2 < 8:
                            pre_na(t + 2)
                        if t < 8:
                            pre_nb(t)
                        if 0 <= t - 1 < 8:
                            pre_t(t - 1)
                        if 0 <= t - 2 < 8:
                            pre_r(t - 2)
                    tt("dve", lg[:], pb[6][:, 0:160].rearrange("p (b f) -> p b f", f=20),
                       br_b[:].unsqueeze(1).to_broadcast([128, 8, 20]), ALU.add, [PB(6), "br_b"], ["lg"])
                    R = lambda *a: list(a)
                    S.op("dve", lambda e: e.tensor_reduce(gmx[:], lg[:, :, 0:4], mybir.AxisListType.X, ALU.max), ["lg"], ["gmx"])
                    tt("dve", oh[:], lg[:, :, 0:4], gmx[:].unsqueeze(2).to_broadcast([128, 8, 4]), ALU.is_ge, ["lg", "gmx"], ["oh"])
                    tt("dve", ge[:], lg[:, :, 0:4], gmx[:].unsqueeze(2).to_broadcast([128, 8, 4]), ALU.subtract, ["lg", "gmx"], ["ge"])
                    act(ge[:], ge[:], AF.Exp, ["ge"], ["ge"])
                    S.op("dve", lambda e: e.tensor_reduce(gs[:], ge[:], mybir.AxisListType.X, ALU.add), ["ge"], ["gs"])
                    rcp(gs[:], gs[:], ["gs"], ["gs"])
                    lge = lg[:, :, 4:20].rearrange("p b (g e) -> p b g e", g=4)
                    tt("dve", esel4[:], lge, oh[:].unsqueeze(3).to_broadcast([128, 8, 4, 4]), ALU.mult, ["lg", "oh"], ["esel4"])
                    S.op("dve", lambda e: e.tensor_reduce(esel[:], esel4[:].rearrange("p b g e -> p b e g"),
                                                          mybir.AxisListType.X, ALU.add), ["esel4"], ["esel"])
                    S.op("dve", lambda e: e.tensor_reduce(e1[:], esel[:], mybir.AxisListType.X, ALU.max), ["esel"], ["e1"])
                    tt("dve", sel[:], esel[:], e1[:].unsqueeze(2).to_broadcast([128, 8, 4]), ALU.is_ge, ["esel", "e1"], ["sel"])
                    stt("dve", em[:], sel[:], -1e30, esel[:], ALU.mult, ALU.add, ["sel", "esel"], ["em"])
                    S.op("dve", lambda e: e.tensor_reduce(e2[:], em[:], mybir.AxisListType.X, ALU.max), ["em"], ["e2"])
                    tt("dve", sel[:], esel[:], e2[:].unsqueeze(2).to_broadcast([128, 8, 4]), ALU.is_ge, ["esel", "e2"], ["sel"])
                    tt("dve", ew[:], esel[:], e1[:].unsqueeze(2).to_broadcast([128, 8, 4]), ALU.subtract, ["esel", "e1"], ["ew"])
                    act(ew[:], ew[:], AF.Exp, ["ew"], ["ew"])
                    tt("dve", ew[:], ew[:], sel[:], ALU.mult, ["ew", "sel"], ["ew"])
                    S.op("dve", lambda e: e.tensor_reduce(es[:], ew[:], mybir.AxisListType.X, ALU.add), ["ew"], ["es"])
                    rcp(es[:], es[:], ["es"], ["es"])
                    tt("dve", es[:], es[:], gs[:], ALU.mult, ["es", "gs"], ["es"])
                    tt("dve", ew[:], ew[:], es[:].unsqueeze(2).to_broadcast([128, 8, 4]), ALU.mult, ["ew", "es"], ["ew"])
                    tt("dve", cmb[:].rearrange("p b (g e) -> p b g e", g=4),
                       oh[:].unsqueeze(3).to_broadcast([128, 8, 4, 4]),
                       ew[:].unsqueeze(2).to_broadcast([128, 8, 4, 4]), ALU.mult, ["oh", "ew"], ["cmb"])

                    wei_v = w_ei.rearrange("e (c p) f -> e p c f", p=128)
                    weo_v = w_eo.rearrange("e (c p) f -> e p c f", p=128)
                    items = [(ex, ii) for ex in range(NE) for ii in range(8)]

                    def moe_a(t):
                        ex, ii = items[t]
                        wk = ex % 2
                        kk = t % 2
                        if ii == 0 and ex >= 2:
                            ldc(wei[:, wk], wei_v[ex], [("wei", wk)])
                            ldc(weo[:, wk], weo_v[ex], [("weo", wk)])
                        hb = kk
                        for c in range(8):
                            mm(pb[hb][:], hnT[:, c, ii * 128:(ii + 1) * 128], wei[:, wk, c, :], c == 0, c == 7,
                               [("hnT", ii), ("wei", wk)], [PB(hb)])
                        act(sg[:, kk, :], pb[hb][:, 0:256], AF.Silu, [PB(hb)], [("sg", kk)])
                        stt("dve", ac[:, kk, :], pb[hb][:, 256:512], cmb[:, ii, ex:ex + 1], sg[:, kk, :], ALU.mult, ALU.mult,
                            [PB(hb), "cmb", ("sg", kk)], [("ac", kk)])

                    def moe_b(t):
                        kk = t % 2
                        tb = 2 + kk
                        tp = pb[tb][:].bitcast(BF16)
                        tr(tp[:, 0:128], ac[:, kk, 0:128], [("ac", kk)], [PB(tb)])
                        tr(tp[:, 128:256], ac[:, kk, 128:256], [("ac", kk)], [PB(tb)])
                        cp("act", acT[:, kk, :], tp[:, 0:256], [PB(tb)], [("acT", kk)])

                    def moe_c(t):
                        ex, ii = items[t]
                        wk = ex % 2
                        kk = t % 2
                        for hf in range(2):
                            yb = 4 + 2 * kk + hf
                            for fc in range(2):
                                mm(pb[yb][:], acT[:, kk, fc * 128:(fc + 1) * 128], weo[:, wk, fc, hf * 512:(hf + 1) * 512],
                                   fc == 0, fc == 1, [("acT", kk), ("weo", wk)], [PB(yb)])
                            tt("dve", hres[:, ii, hf * 512:(hf + 1) * 512], pb[yb][:], hres[:, ii, hf * 512:(hf + 1) * 512],
                               ALU.add, [PB(yb), ("hres", ii, hf)], [("hres", ii, hf)])

                    NI = len(items)
                    for t in range(NI + 2):
                        if t < NI:
                            moe_a(t)
                        if 0 <= t - 1 < NI:
                            moe_b(t - 1)
                        if 0 <= t - 2 < NI:
                            moe_c(t - 2)
                    if stop == "E" and half == 0:
                        dump("hres", hres[:], [128, 8, D], F32, [("hres", b) for b in range(8)])
                        dump("cmb", cmb[:], [128, 8, 16], F32, ["cmb"])
                        return finish()
                    S.barrier()
                    S.flush()

                with ExitStack() as pst:
                    wpg = sb("wpg", [128, 8, D], BF16, pst)
                    wpp = sb("wpp", [128, 2, D], BF16, pst)
                    pTs = sb("pTs", [128, 2, 8 * 128], BF16, pst)
                    hn3 = sb("hn3", [128, 2, D], BF16, pst)
                    hn3T = sb("hn3T", [128, 2, D], BF16, pst)
                    junk3 = sb("junk3", [128, D], F32, pst)
                    ss3 = sb("ss3", [128, 8], F32, pst)
                    ss4 = sb("ss4", [128, 8], F32, pst)
                    th3 = sb("th3", [128, 2, D], F32, pst)
                    gple_b = sb("gple_b", [128, D], F32, pst)
                    gfin_b = sb("gfin_b", [128, D], F32, pst)
                    ld(gple_b[:], g_ple.partition_broadcast(128), ["gple_b"])
                    ld(gfin_b[:], g_fin.partition_broadcast(128), ["gfin_b"])
                    ldc(wpg[:], w_pg.rearrange("(c p) f -> p c f", p=128), ["wpg"])
                    ldc(wpp[:], w_pp.rearrange("(c p) f -> p c f", p=128), ["wpp"])
                    ldc(pTs[:], pT_own.rearrange("(c p) t -> p c t", p=128)[:, :, half * 1024:(half + 1) * 1024], ["pTs"])
                    ms("dve", ss3[:], EPS, [("ss3", ii_) for ii_ in range(8)])
                    ms("dve", ss4[:], EPS, [("ss4", ii_) for ii_ in range(8)])

                    def ple_na(ii):
                        act(junk3[:], hres[:, ii, :], AF.Square, [("hres", ii)], ["junk3", ("ss3", ii)], accum=ss3[:, ii:ii + 1],
                            scale=float(D ** -0.5))
                        rstd(ss3[:, ii:ii + 1], ss3[:, ii:ii + 1], 1, [("ss3", ii)], [("ss3", ii)])

                    def ple_nb(ii):
                        kk = ii % 2
                        stt("dve", hn3[:, kk, :], hres[:, ii, :], ss3[:, ii:ii + 1], gple_b[:], ALU.mult, ALU.mult,
                            [("hres", ii), ("ss3", ii), "gple_b"], [("hn3", kk)])

                    def ple_t(ii):
                        kk = ii % 2
                        tbank = 0 + kk
                        tp = pb[tbank][:].bitcast(BF16)
                        for c in range(8):
                            tr(tp[:, c * 128:(c + 1) * 128], hn3[:, kk, c * 128:(c + 1) * 128], [("hn3", kk)], [PB(tbank)])
                        cp("act", hn3T[:, kk, :], tp[:, :], [PB(tbank)], [("hn3T", kk)])

                    def ple_g(ii):
                        i = half * 8 + ii
                        kk = ii % 2
                        for hf in range(2):
                            gbk = 2 + hf
                            for c in range(8):
                                mm(pb[gbk][:], hn3T[:, kk, c * 128:(c + 1) * 128], wpg[:, c, hf * 512:(hf + 1) * 512],
                                   c == 0, c == 7, [("hn3T", kk), "wpg"], [PB(gbk)])
                            act(th3[:, kk, hf * 512:(hf + 1) * 512], pb[gbk][:], AF.Tanh, [PB(gbk)], [("th3", kk, hf)], scale=0.5)
                            pbk = 4 + hf
                            for c in range(2):
                                mm(pb[pbk][:], pTs[:, c, ii * 128:(ii + 1) * 128], wpp[:, c, hf * 512:(hf + 1) * 512],
                                   c == 0, c == 1, ["pTs", "wpp"], [PB(pbk)])
                            th_ = th3[:, kk, hf * 512:(hf + 1) * 512]
                            stt("dve", th_, th_, 1.0, pb[pbk][:], ALU.add, ALU.mult, [("th3", kk, hf), PB(pbk)], [("th3", kk, hf)])
                            stt("dve", th_, th_, 0.5, hres[:, ii, hf * 512:(hf + 1) * 512], ALU.mult, ALU.add,
                                [("th3", kk, hf), ("hres", ii)], [("th3", kk, hf)])
                        act(junk3[:], th3[:, kk, :], AF.Square, [("th3", kk, 0), ("th3", kk, 1)], ["junk3", ("ss4", ii)],
                            accum=ss4[:, ii:ii + 1], scale=float(D ** -0.5))
                        rstd(ss4[:, ii:ii + 1], ss4[:, ii:ii + 1], 1, [("ss4", ii)], [("ss4", ii)])

                    def ple_g2(ii):
                        i = half * 8 + ii
                        kk = ii % 2
                        stt("dve", th3[:, kk, :], th3[:, kk, :], ss4[:, ii:ii + 1], gfin_b[:], ALU.mult, ALU.mult,
                            [("th3", kk, 0), ("th3", kk, 1), ("ss4", ii), "gfin_b"], [("th3", kk, 0), ("th3", kk, 1)])
                        ld(out_own[i * 128:(i + 1) * 128, :], th3[:, kk, :], [("out", i)], r=[("th3", kk, 0), ("th3", kk, 1)])

                    ple_na(0)
                    for t in range(8 + 3):
                        if 0 <= t - 3 < 8:
                            ple_g2(t - 3)
                        if t + 1 < 8:
                            ple_na(t + 1)
                        if t < 8:
                            ple_nb(t)
                        if 0 <= t - 1 < 8:
                            ple_t(t - 1)
                        if 0 <= t - 2 < 8:
                            ple_g(t - 2)
                    S.barrier()
                    S.flush()
    return nc


_NC_CACHE = {}


def _rope_tables():
    pos = np.arange(SEQ, dtype=np.float32)
    inv = (np.float32(10000.0) ** (-np.arange(0, 64, 2, dtype=np.float32) / np.float32(64))).astype(np.float32)
    ang = (pos[:, None] * inv[None, :]).astype(np.float32)
    return np.cos(ang).astype(np.float32), np.sin(ang).astype(np.float32)


def _prepare(x, p, g_mix, w_in, swa_sinks, mla_g_q, mla_w_uq, mla_g_kv, mla_w_ukv, w_out,
           g_ffn, w_router_group, b_router_group, w_router_expert, b_router_expert,
           w_expert_in, w_expert_out, g_ple, w_ple_gate, w_ple_proj, g_final):
    f = lambda a: np.ascontiguousarray(np.asarray(a, dtype=np.float32))
    x = f(x); p = f(p)
    B = x.shape[0]
    cos, sin = _rope_tables()
    cs_all = np.concatenate([cos, sin], axis=1)
    shared = {
        "cs_all": f(cs_all),
        "w_in": f(w_in[0]), "w_uq": f(mla_w_uq[0]), "w_ukv": f(mla_w_ukv[0]),
        "w_ukvT": f(np.asarray(mla_w_ukv[0]).T), "w_out": f(w_out[0]),
        "w_r": f(np.concatenate([np.asarray(w_router_group[0]), np.asarray(w_router_expert[0])], axis=1)),
        "b_r": f(np.concatenate([np.asarray(b_router_group[0]), np.asarray(b_router_expert[0])], axis=0)),
        "w_ei": f(w_expert_in[0]), "w_eo": f(w_expert_out[0]),
        "w_pg": f(w_ple_gate[0]), "w_pp": f(w_ple_proj[0]),
        "g_mixT": f(np.asarray(g_mix[0]).reshape(8, 128).T),
        "g_q": f(mla_g_q[0]), "g_kv": f(mla_g_kv[0]), "g_ffn": f(g_ffn[0]), "g_ple": f(g_ple[0]),
        "g_fin": f(g_final), "sinks": f(swa_sinks[0]),
    }
    kq = np.arange(128)
    tri_le = (kq[:, None] <= kq[None, :]).astype(np.float32)
    tri_gt = (kq[:, None] > kq[None, :]).astype(np.float32)
    in_maps = []
    own_rows = []
    for c in range(8):
        b, j = c // 4, c % 4
        blocks = np.array([4 * i + j for i in range(NOWN)])
        own_tok = (blocks[:, None] * 128 + np.arange(128)[None, :]).reshape(-1)
        prev_tok = own_tok.reshape(NOWN, 128) - 128
        pair_tok = np.concatenate([prev_tok, own_tok.reshape(NOWN, 128)], axis=1)
        valid = (pair_tok >= 0)
        pair_idx = np.where(valid, pair_tok, 0).reshape(-1)
        xb = x[b]
        x_pair = xb[pair_idx].copy()
        x_pair[~valid.reshape(-1)] = 0.0
        csT = np.zeros((128, 2, NOWN * 128), np.float32)
        csT[0:32, 0] = cos[own_tok].T; csT[32:64, 0] = cos[own_tok].T
        csT[0:32, 1] = sin[own_tok].T; csT[32:64, 1] = sin[own_tok].T
        cst = np.zeros((8, 128, 128), np.float32)
        cst[0] = np.eye(128, dtype=np.float32)
        for m in range(4):
            cst[1 + m] = 1.0 if m < j else (tri_le if m == j else 0.0)
        cst[5] = 0.0 if j == 0 else tri_gt
        cst[6] = tri_gt
        cst[7] = tri_le
        d = dict(shared)
        d.update({
            "xT_full": f(xb.T), "xT_q": f(xb[own_tok].T), "xT_pair": f(x_pair.T), "x_own": f(xb[own_tok]),
            "pT_own": f(p[0, b][own_tok].T), "csT_own": f(csT), "cs_pair": f(cs_all[pair_idx]),
            "consts": f(cst.transpose(1, 0, 2)),
        })
        in_maps.append(d)
        own_rows.append((b, own_tok))
    return in_maps, own_rows, B


def kernel(**inputs):
    in_maps, own_rows, B = _prepare(**inputs)
    if "nc" not in _NC_CACHE:
        _NC_CACHE["nc"] = build_program()
    res = run_bass_kernel_spmd(_NC_CACHE["nc"], in_maps, core_ids=list(range(8)))
    out = np.zeros((B, SEQ, D), np.float32)
    for c in range(8):
        b, own_tok = own_rows[c]
        out[b, own_tok] = np.asarray(res.results[c]["out_own"], dtype=np.float32)
    return out
```
